# Optimizing a Trainium2 kernel written in Bass

```python
import math
import jax, jax.numpy as jnp
from jax import lax
import numpy as np

D_MODEL = 1024
BATCH = 16
SEQ = 4096
DEPTH = 4

N_MIXERS = 2
N_LRU = (DEPTH + 1) // 2
N_RET = DEPTH // 2

D_RNN = 3 * D_MODEL // 2
N_GATE_BLOCKS = 16
BLOCK_W = D_RNN // N_GATE_BLOCKS
CONV_W = 4
C_RG = 8.0

RET_HEADS = D_MODEL // 256
QK_HEAD = 256
V_HEAD = 2 * QK_HEAD
QK_DIM = RET_HEADS * QK_HEAD
V_DIM = RET_HEADS * V_HEAD
CHUNK = 128
ROPE_BASE = 10000.0

RMS_EPS = 1e-6
GN_EPS = 1e-5

kernel_name = "hybrid_rglru_retention_sandwich"


def rms_norm(x, g):
    xf = x.astype(jnp.float32)
    y = xf * lax.rsqrt(jnp.mean(xf * xf, axis=-1, keepdims=True) + RMS_EPS)
    return (y * g.astype(jnp.float32)).astype(x.dtype)


def causal_depthwise_conv(x, w, b):
    s = x.shape[1]
    xp = jnp.pad(x, ((0, 0), (CONV_W - 1, 0), (0, 0)))
    y = b
    for k in range(CONV_W):
        y = y + xp[:, k:k + s] * w[k]
    return y


def _lin_rec_combine(e1, e2):
    a1, b1 = e1
    a2, b2 = e2
    return a1 * a2, a2 * b1 + b2


def rglru_mixer(h, w_in, conv_w, conv_b, w_a, b_a, w_x, b_x, lam, w_out):
    bsz, s, _ = h.shape
    u = h @ w_in
    xb, gate = u[..., :D_RNN], u[..., D_RNN:]
    xb = causal_depthwise_conv(xb, conv_w, conv_b)
    xblk = xb.reshape(bsz, s, N_GATE_BLOCKS, BLOCK_W)
    r = jax.nn.sigmoid(jnp.einsum('bsnc,ncd->bsnd', xblk, w_a).reshape(bsz, s, D_RNN) + b_a)
    i = jax.nn.sigmoid(jnp.einsum('bsnc,ncd->bsnd', xblk, w_x).reshape(bsz, s, D_RNN) + b_x)
    log_a = -C_RG * r.astype(jnp.float32) * jax.nn.softplus(-lam.astype(jnp.float32))
    a = jnp.exp(log_a)
    bt = jnp.sqrt(-jnp.expm1(2.0 * log_a)) * (i * xb).astype(jnp.float32)
    _, hseq = lax.associative_scan(_lin_rec_combine, (a, bt), axis=1)
    y = hseq.astype(h.dtype) * jax.nn.silu(gate)
    return y @ w_out


def apply_rotary(x, cos, sin):
    half = x.shape[-1] // 2
    x1, x2 = x[..., :half], x[..., half:]
    c = cos[None, :, None, :]
    s_ = sin[None, :, None, :]
    return jnp.concatenate([x1 * c - x2 * s_, x2 * c + x1 * s_], axis=-1)


def retention_mixer(h, w_in, w_out, cos, sin):
    bsz, s, _ = h.shape
    n_chunks = s // CHUNK
    u = h @ w_in
    q = u[..., :QK_DIM].reshape(bsz, s, RET_HEADS, QK_HEAD).astype(jnp.float32)
    k = u[..., QK_DIM:2 * QK_DIM].reshape(bsz, s, RET_HEADS, QK_HEAD).astype(jnp.float32)
    v = u[..., 2 * QK_DIM:2 * QK_DIM + V_DIM].reshape(bsz, s, RET_HEADS, V_HEAD).astype(jnp.float32)
    gate = u[..., 2 * QK_DIM + V_DIM:]
    q = apply_rotary(q, cos, sin)
    k = apply_rotary(k, cos, sin) * (QK_HEAD ** -0.5)

    def to_chunks(t):
        return t.reshape(bsz, n_chunks, CHUNK, RET_HEADS, t.shape[-1]).transpose(1, 0, 3, 2, 4)

    qc, kc, vc = to_chunks(q), to_chunks(k), to_chunks(v)

    log_g = jnp.log(1.0 - jnp.exp2(-5.0 - jnp.arange(RET_HEADS, dtype=jnp.float32)))
    idx = jnp.arange(CHUNK, dtype=jnp.float32)
    diff = idx[:, None] - idx[None, :]
    dmask = jnp.where(diff[None] >= 0, jnp.exp(jnp.maximum(diff, 0.0)[None] * log_g[:, None, None]), 0.0)
    q_decay = jnp.exp((idx[None, :] + 1.0) * log_g[:, None])[..., None]
    k_decay = jnp.exp((CHUNK - 1.0 - idx[None, :]) * log_g[:, None])[..., None]
    chunk_decay = jnp.exp(CHUNK * log_g)[:, None, None]

    def step(state, inp):
        qi, ki, vi = inp
        scores = jnp.einsum('bhqd,bhkd->bhqk', qi, ki) * dmask
        inner = jnp.einsum('bhqk,bhke->bhqe', scores, vi)
        cross = jnp.einsum('bhqd,bhde->bhqe', qi * q_decay, state)
        new_state = state * chunk_decay + jnp.einsum('bhkd,bhke->bhde', ki * k_decay, vi)
        return new_state, inner + cross

    state0 = jnp.zeros((bsz, RET_HEADS, QK_HEAD, V_HEAD), jnp.float32)
    _, out = lax.scan(step, state0, (qc, kc, vc))
    out = out.transpose(1, 0, 3, 2, 4).reshape(bsz, s, RET_HEADS, V_HEAD)
    mu = jnp.mean(out, axis=-1, keepdims=True)
    var = jnp.mean(jnp.square(out - mu), axis=-1, keepdims=True)
    out = ((out - mu) * lax.rsqrt(var + GN_EPS)).reshape(bsz, s, V_DIM).astype(h.dtype)
    y = out * jax.nn.silu(gate)
    return y @ w_out


def setup_inputs(seed: int = 0) -> dict:
    key = jax.random.key(seed)
    ks = jax.random.split(key, 16)
    f32 = jnp.float32

    def nrm(k, shape, fan_in):
        return jax.random.normal(k, shape, f32) * (fan_in ** -0.5)

    x = jax.random.normal(ks[0], (BATCH, SEQ, D_MODEL), f32)
    norm_pre = 1.0 + 0.05 * jax.random.normal(ks[1], (DEPTH, D_MODEL), f32)
    norm_post = 1.0 + 0.05 * jax.random.normal(ks[2], (DEPTH, D_MODEL), f32)
    lru_w_in = nrm(ks[3], (N_LRU, D_MODEL, 2 * D_RNN), D_MODEL)
    lru_conv_w = nrm(ks[4], (N_LRU, CONV_W, D_RNN), CONV_W)
    lru_conv_b = 0.02 * jax.random.normal(ks[5], (N_LRU, D_RNN), f32)
    lru_w_a = nrm(ks[6], (N_LRU, N_GATE_BLOCKS, BLOCK_W, BLOCK_W), BLOCK_W)
    lru_b_a = 0.02 * jax.random.normal(ks[7], (N_LRU, D_RNN), f32)
    lru_w_x = nrm(ks[8], (N_LRU, N_GATE_BLOCKS, BLOCK_W, BLOCK_W), BLOCK_W)
    lru_b_x = 0.02 * jax.random.normal(ks[9], (N_LRU, D_RNN), f32)
    a_c = jax.random.uniform(ks[10], (N_LRU, D_RNN), f32, minval=0.9, maxval=0.999)
    sig = a_c ** (1.0 / C_RG)
    lru_lambda = jnp.log(sig) - jnp.log1p(-sig)
    lru_w_out = nrm(ks[11], (N_LRU, D_RNN, D_MODEL), D_RNN)
    ret_w_in = nrm(ks[12], (N_RET, D_MODEL, 2 * QK_DIM + 2 * V_DIM), D_MODEL)
    ret_w_out = nrm(ks[13], (N_RET, V_DIM, D_MODEL), V_DIM)
    return {"x": x, "norm_pre": norm_pre, "norm_post": norm_post,
            "lru_w_in": lru_w_in, "lru_conv_w": lru_conv_w, "lru_conv_b": lru_conv_b,
            "lru_w_a": lru_w_a, "lru_b_a": lru_b_a, "lru_w_x": lru_w_x, "lru_b_x": lru_b_x,
            "lru_lambda": lru_lambda, "lru_w_out": lru_w_out,
            "ret_w_in": ret_w_in, "ret_w_out": ret_w_out}


def reference(x, norm_pre, norm_post, lru_w_in, lru_conv_w, lru_conv_b, lru_w_a, lru_b_a,
              lru_w_x, lru_b_x, lru_lambda, lru_w_out, ret_w_in, ret_w_out):
    s = x.shape[1]
    pos = jnp.arange(s, dtype=jnp.float32)
    inv_freq = ROPE_BASE ** (-jnp.arange(0, QK_HEAD, 2, dtype=jnp.float32) / QK_HEAD)
    ang = pos[:, None] * inv_freq[None, :]
    cos, sin = jnp.cos(ang), jnp.sin(ang)
    for layer in range(DEPTH):
        h = rms_norm(x, norm_pre[layer])
        j = layer // N_MIXERS
        if layer % N_MIXERS == 0:
            o = rglru_mixer(h, lru_w_in[j], lru_conv_w[j], lru_conv_b[j], lru_w_a[j], lru_b_a[j],
                            lru_w_x[j], lru_b_x[j], lru_lambda[j], lru_w_out[j])
        else:
            o = retention_mixer(h, ret_w_in[j], ret_w_out[j], cos, sin)
        x = x + rms_norm(o, norm_post[layer])
    return x
```

```python
from contextlib import ExitStack
import math
import os
import numpy as np
import ml_dtypes
import concourse.bass as bass
import concourse.mybir as mybir
from concourse.bass_utils import run_bass_kernel_spmd

F32 = mybir.dt.float32
BF16 = mybir.dt.bfloat16
ALU = mybir.AluOpType
AF = mybir.ActivationFunctionType

D = 1024
DR = 1536
T = 512
NSUB = 4
NCORE = 8
RMS_EPS = 1e-6
GN_EPS = 1e-5
BL = [(0, 0), (0, 1), (1, 0), (1, 1), (1, 2), (2, 1), (2, 2)]
GAM = [1.0 - 2.0 ** (-5.0 - h) for h in range(4)]
QCLAMP = float(np.nextafter(np.float32(0.25), np.float32(0.0)))


class Buf:
    __slots__ = ("name", "arena", "lo", "hi", "w", "r", "ov")

    def __init__(self, name, arena=None, lo=0, hi=0):
        self.name, self.arena, self.lo, self.hi = name, arena, lo, hi
        self.w = None
        self.r = set()
        self.ov = [self]


class _Dry:
    def __init__(self, eng):
        self.eng, self.cost, self.tbl = eng, 0.0, None

    @staticmethod
    def _n(ap):
        sh = ap.shape
        n = 1
        for d in sh[1:]:
            n *= int(d)
        return n

    def then_inc(self, *a, **k):
        return self

    def __getattr__(self, name):
        def f(*a, **k):
            if name == "matmul":
                self.cost += max(self._n(k["rhs"]), 64) / 2400.0 * 1.2
            elif name == "transpose":
                self.cost += 0.075
            elif name == "dma_start":
                o = k["out"]
                nb = self._n(o) * int(o.shape[0]) * (2 if o.dtype == BF16 else 4)
                self.cost += 2.0 + nb / 150e3
            elif name == "activation":
                self.cost += (self._n(k["out"]) + 230) / 1200.0 + (0.1 if k.get("accum_out") is not None else 0.0)
                fnc = k["func"]
                if fnc in (AF.Exp, AF.Tanh):
                    self.tbl = "exp"
                elif fnc == AF.Sqrt:
                    self.tbl = "sqrt"
                elif fnc == AF.Ln:
                    self.tbl = "ln"
            else:
                o = k.get("out", a[0] if a else None)
                n = self._n(k["in_"]) if name == "bn_stats" else self._n(o)
                if self.eng == "gpsimd":
                    if name == "memset":
                        self.cost += 0.3 + n * 0.0003
                    else:
                        self.cost += 0.2 + n * 0.015 + (0.3 if k.get("op") == ALU.pow else 0.0)
                else:
                    self.cost += (n + 150) / 960.0
            return self
        return f


class Prog:
    ENG = ("tensor", "vector", "scalar", "gpsimd", "sync")

    def __init__(self):
        self.ops = []
        self.arena_bufs = {}
        self.tag = ""
        self.dma_k = {"cst": 8, "prep": 6}
        self.q = None
        self.cnt = None

    def buf(self, name, arena=None, lo=0, hi=0):
        b = Buf(name, arena, lo, hi)
        b.r = set()
        if arena is not None:
            lst = self.arena_bufs.setdefault(arena, [])
            for o in lst:
                if o.lo < hi and lo < o.hi:
                    o.ov.append(b)
                    b.ov.append(o)
            lst.append(b)
        return b

    def op(self, eng, fn, reads=(), writes=(), dma=None):
        idx = len(self.ops)
        preds = set()
        for b in reads:
            for o in b.ov:
                if o.w is not None:
                    preds.add(o.w)
        for b in writes:
            for o in b.ov:
                if o.w is not None:
                    preds.add(o.w)
                preds |= o.r
        preds.discard(idx)
        d = _Dry(eng)
        if fn is not None:
            fn(d)
        self.ops.append([eng, fn, dma, preds, d.cost, d.tbl, self.tag])
        for b in reads:
            b.r.add(idx)
        for b in writes:
            b.w = idx
            b.r = set()
        return idx

    def wait_all(self, eng, streams):
        preds = set(i for i, o in enumerate(self.ops) if o[2] in streams)
        self.ops.append([eng, None, None, preds, 0.0, None, "end"])

    def finalize(self, reorder=True):
        import heapq
        ops = self.ops
        n = len(ops)
        succ = [[] for _ in range(n)]
        indeg = [0] * n
        for i, o in enumerate(ops):
            indeg[i] = len(o[3])
            for p in o[3]:
                succ[p].append(i)
        order = []
        if reorder:
            fin = [0.0] * n
            efree = {e: 0.0 for e in self.ENG}
            etbl = {e: None for e in self.ENG}
            wait = {e: [] for e in self.ENG}
            avail = {e: [] for e in self.ENG}
            for i in range(n):
                if indeg[i] == 0:
                    heapq.heappush(wait[ops[i][0]], (0.0, i))
            done = 0
            while done < n:
                best = None
                for e in self.ENG:
                    w, a = wait[e], avail[e]
                    while w and w[0][0] <= efree[e]:
                        heapq.heappush(a, heapq.heappop(w)[1])
                    if a:
                        cand = (efree[e], a[0], e, True)
                    elif w:
                        cand = (w[0][0], w[0][1], e, False)
                    else:
                        continue
                    if best is None or cand[:2] < best[:2]:
                        best = cand
                st, i, e, from_a = best
                if from_a:
                    if e == "scalar" and ops[i][5] is not None and ops[i][5] != etbl[e] and len(avail[e]) > 1:
                        small = heapq.nsmallest(6, avail[e])
                        alt = [j for j in small if ops[j][5] is None or ops[j][5] == etbl[e]]
                        if alt and alt[0] - i < 400:
                            i = alt[0]
                            avail[e].remove(i)
                            heapq.heapify(avail[e])
                        else:
                            heapq.heappop(avail[e])
                    else:
                        heapq.heappop(avail[e])
                else:
                    heapq.heappop(wait[e])
                o = ops[i]
                dur = o[4]
                if e == "scalar" and o[5] is not None and o[5] != etbl[e]:
                    dur += 1.3
                    etbl[e] = o[5]
                if o[2]:
                    efree[e] = st + 0.06
                    fin[i] = st + dur
                else:
                    efree[e] = st + dur + 0.06
                    fin[i] = st + dur + 0.15
                order.append(i)
                done += 1
                for sct in succ[i]:
                    indeg[sct] -= 1
                    if indeg[sct] == 0:
                        rt = 0.0
                        for p in ops[sct][3]:
                            if fin[p] > rt:
                                rt = fin[p]
                        heapq.heappush(wait[ops[sct][0]], (rt, sct))
            self.est_us = max(fin) if fin else 0.0
        else:
            order = list(range(n))
        self.q = {e: [] for e in self.ENG}
        self.tags = {e: [] for e in self.ENG}
        cnt = {}
        seen = {e: {} for e in self.ENG}
        ev = [None] * n
        dma_n = {}
        dma_hist = {}
        for i in order:
            eng, fn, dma, preds, dur, tbl, tag = ops[i]
            deps = {}
            sn = seen[eng]
            for p in preds:
                k, v = ev[p]
                if eng == "tensor" and k == "tensor":
                    continue
                if sn.get(k, 0) >= v:
                    continue
                if deps.get(k, 0) < v:
                    deps[k] = v
            if fn is None:
                self.q[eng].append((tuple(deps.items()), None, None, 0))
                self.tags[eng].append(tag)
                ev[i] = (eng, cnt.get(eng, 0))
                continue
            if dma:
                m = dma_n.get(dma, 0)
                dma_n[dma] = m + 1
                K = self.dma_k.get(dma, 4)
                key, inc = "%s.%d" % (dma, m % K), 16
                if m >= K and sn.get(key, 0) < 16 * (m // K):
                    deps[key] = max(deps.get(key, 0), 16 * (m // K))
            else:
                key, inc = eng, 1
            for k, v in deps.items():
                sn[k] = v
            val = cnt.get(key, 0) + inc
            cnt[key] = val
            ev[i] = (key, val)
            self.q[eng].append((tuple(deps.items()), fn, key, inc))
            self.tags[eng].append(tag)
        self.cnt = cnt


def build(NSEQ, SEQ, layers, dbg_specs=None):
    NT = SEQ // T
    nc = bass.Bass("TRN2", target_bir_lowering=False)
    P = Prog()
    es = ExitStack()

    def din(name, shape, dt=F32):
        return nc.dram_tensor(name, list(shape), dt, kind="ExternalInput").ap()

    def dscr(name, shape, dt=BF16):
        return nc.dram_tensor(name, list(shape), dt, kind="Internal").ap()

    x_d = din("x", [NSEQ * SEQ, D])
    out_d = nc.dram_tensor("out", [NSEQ * SEQ, D], F32, kind="ExternalOutput").ap()
    gpre_d = din("gpre", [4, 128, D])
    gpost_d = din("gpost", [4, 128, D])
    lin_d = din("lru_in", [2, 8, 128, 3072])
    lout_d = din("lru_out", [2, 4, 128, 3072])
    lband_d = din("lru_band", [2, 128, 2 * 3584])
    lvec_d = din("lru_vec", [2, 128, 12 * 9])
    rin_d = din("ret_in", [2, 12, 128, 4096])
    rout_d = din("ret_out", [2, 4, 128, 4096])
    cos_d = din("cosT", [128, SEQ])
    sin_d = din("sinT", [128, SEQ])
    rcon_d = din("rcon", [128, 512 + 1024 + 4])
    ident_d = din("ident", [128, 128], BF16)

    lin_s = dscr("lin_s", [2, 8, 128, 3072])
    lout_s = dscr("lout_s", [2, 4, 128, 3072])
    lband_s = dscr("lband_s", [2, 128, 2 * 3584])
    rin_s = dscr("rin_s", [2, 12, 128, 4096])
    rout_s = dscr("rout_s", [2, 4, 128, 4096])

    def sb(name, shape, dt=F32):
        return es.enter_context(nc.sbuf_tensor("sb_" + name, list(shape), dt))

    def ps(name, shape, dt=F32):
        return es.enter_context(nc.psum_tensor("ps_" + name, list(shape), dt))

    xt = sb("xt", [128, NSUB, D]);            b_xt = [P.buf("xt%d" % j) for j in range(NSUB)]
    hn2 = [sb("hn%d" % i, [128, D], BF16) for i in range(2)]
    b_hn2 = [P.buf("hn%d" % i) for i in range(2)]
    junk = sb("junk", [128, D], BF16);        b_junk = P.buf("junk")
    hT = sb("hT", [128, 8, T], BF16);         b_hT = P.buf("hT")
    gpre = sb("gpre", [128, D]);              b_gpre = P.buf("gpre")
    gpost = sb("gpost", [128, D]);            b_gpost = P.buf("gpost")
    NW = 3
    wr = [sb("wr%d" % i, [128, 4096], BF16) for i in range(NW)]
    b_wr = [P.buf("wr%d" % i) for i in range(NW)]
    yT = sb("yT", [128, 16, T], BF16);        b_yT = [P.buf("yT%d" % c) for c in range(16)]
    otmp = sb("otmp", [128, NSUB, 512]);      b_otmp = [P.buf("otmp%d" % j) for j in range(NSUB)]
    ident = sb("ident", [128, 128], BF16);    b_ident = P.buf("ident")
    stat = sb("stat", [128, 64]);
    b_stat = [P.buf("stat%d" % i) for i in range(64)]
    S32 = [sb("S32_%d" % i, [128, 8, 512]) for i in range(2)]
    b_S32 = [[P.buf("S32_%d_%d" % (i, k)) for k in range(8)] for i in range(2)]
    lvec = [sb("lvec%d" % i, [128, 12, 9]) for i in range(2)]
    b_lvec = [P.buf("lvec%d" % i) for i in range(2)]
    lder = [sb("lder%d" % i, [128, 12, 6]) for i in range(2)]
    b_lder = [P.buf("lder%d" % i) for i in range(2)]
    hst = [sb("hst%d" % i, [128, 12]) for i in range(2)]
    b_hst = [[P.buf("hst%d_%d" % (i, c)) for c in range(12)] for i in range(2)]
    halo = [sb("halo%d" % i, [128, 12, 3]) for i in range(2)]
    b_halo = [[P.buf("halo%d_%d" % (i, g)) for g in range(4)] for i in range(2)]
    wband = sb("wband", [128, 2, 4 * 7 * 128], BF16); b_wband = P.buf("wband")
    rcon = sb("rcon", [128, 512 + 1024 + 4]);         b_rcon = P.buf("rcon")
    maskT = rcon[:, 0:512]
    kdecb = rcon[:, 512:1536]
    epsc = rcon[:, 1536:1540]

    cpow = sb("cpow", [128, 520]);                     b_cpow = P.buf("cpow")
    ARW = 16512
    arena = sb("arena", [128, ARW])

    class Scope:
        def __init__(self):
            self.off = 0

        def f32(self, name, shape):
            n = int(np.prod(shape))
            lo = self.off
            self.off += n
            assert self.off <= ARW, (name, self.off)
            ap = arena[:, lo:lo + n]
            if len(shape) == 2:
                ap = ap.rearrange("p (a b) -> p a b", b=shape[1])
            return ap, P.buf(name, "ar", lo, lo + n)

        def bf(self, name, shape):
            n = int(np.prod(shape))
            w = (n + 1) // 2
            lo = self.off
            self.off += w
            assert self.off <= ARW, (name, self.off)
            ap = arena[:, lo:lo + w].bitcast(BF16)
            if len(shape) == 2:
                ap = ap.rearrange("p (a b) -> p a b", b=shape[1])
            elif len(shape) == 3:
                ap = ap.rearrange("p (a b c) -> p a b c", b=shape[1], c=shape[2])
            return ap, P.buf(name, "ar", lo, lo + w)

    L = Scope()
    xbraw, b_xbraw = zip(*[L.f32("xbraw%d" % i, [3, 516]) for i in range(1)])
    xc, b_xc = zip(*[L.f32("xc%d" % i, [3, 512]) for i in range(2)])
    xcbf, b_xcbf = zip(*[L.bf("xcbf%d" % i, [3, 512]) for i in range(2)])
    LT = {}
    NB = 5
    for nm in ("tr", "ti", "a", "m", "tg", "wg"):
        LT[nm] = [L.f32("l_%s%d" % (nm, i), [512]) for i in range({"ti": NB, "a": NB, "m": 3, "wg": 3}.get(nm, 2))]
    for i in range(2):
        bx = P.buf("yx%d" % i)
        b_yT[12 + 2 * i] = b_yT[13 + 2 * i] = bx
        LT["m"].append((yT[:, 12 + 2 * i:14 + 2 * i, :].bitcast(F32).rearrange("p a b -> p (a b)"), bx))
    LT["u"] = [(otmp[:, i, :], b_otmp[i]) for i in range(2)]
    LT["hs"] = [(otmp[:, 2 + i, :], b_otmp[2 + i]) for i in range(2)]
    R = Scope()
    qT, b_qT = R.bf("qT", [4, 512])
    kT, b_kT = R.bf("kT", [4, 512])
    vtok, b_vtok = R.bf("vtok", [NSUB, 1024])
    ktok, b_ktok = R.bf("ktok", [NSUB, 512])
    sgate, b_sgate = R.bf("sgate", [8, 512])
    Sbf, b_Sbf_all = R.bf("Sbf", [8, 512])
    b_Sbf = [P.buf("Sbf%d" % k, "ar", b_Sbf_all.lo + k * 256, b_Sbf_all.lo + (k + 1) * 256) for k in range(8)]
    cosb, b_cos = R.f32("cos", [512])
    sinb, b_sin = R.f32("sin", [512])
    PT, b_PT = zip(*[R.bf("PT%d" % i, [2, 128]) for i in range(2)])
    gntok, b_gntok = zip(*[R.bf("gntok%d" % i, [1024]) for i in range(4)])
    rtg, b_rtg = zip(*[R.f32("rtg%d" % i, [512]) for i in range(2)])
    rt, b_rt = zip(*[R.f32("rt%d" % i, [512]) for i in range(4)])

    pf = [ps("pf%d" % i, [128, 512]) for i in range(7)]
    b_pf = [P.buf("pf%d" % i) for i in range(7)]
    ptb = ps("ptb", [128, 1024], BF16)
    b_ptbh = [P.buf("ptb")]
    pf.append(ptb[:].bitcast(F32))
    b_pf.append(b_ptbh[0])
    pf3_bf = pf[3][:].bitcast(BF16)

    class Rot:
        def __init__(self, idx):
            self.idx, self.i = idx, 0

        def next(self):
            k = self.idx[self.i % len(self.idx)]
            self.i += 1
            return pf[k], b_pf[k]

    V, S_, G, PE, SY = "vector", "scalar", "gpsimd", "tensor", "sync"

    def dma(eng, stream, out, in_, reads=(), writes=()):
        P.op(eng, lambda e: e.dma_start(out=out, in_=in_), reads=reads, writes=writes, dma=stream)

    b_scr = {}

    def prep(name, dst, src):
        b = P.buf(name)
        b_scr[name] = b
        dma(G, "prep", dst, src, writes=[b])

    wload_n = [0]

    def wload(name, src, width):
        i = wload_n[0] % NW
        wload_n[0] += 1
        dma(SY, "wl", wr[i][:, 0:width], src, reads=[b_scr[name]], writes=[b_wr[i]])
        return wr[i], b_wr[i]

    P.op(G, lambda e: e.memset(cpow[:, 0:8], -0.5), writes=[b_cpow])
    P.op(G, lambda e: e.memset(cpow[:, 8:520], 0.5), writes=[b_cpow])
    dma(G, "cstg", ident[:], ident_d, writes=[b_ident])
    dma(G, "cstg", rcon[:], rcon_d, writes=[b_rcon])
    for l in layers:
        j = l // 2
        if l % 2 == 0:
            dma(G, "cstg", lvec[j][:], lvec_d[j].rearrange("p (c k) -> p c k", k=9), writes=[b_lvec[j]])
    done_w = set()
    for l in layers:
        j = l // 2
        if (l % 2, j) in done_w:
            continue
        done_w.add((l % 2, j))
        if l % 2 == 0:
            prep("lband%d" % j, lband_s[j], lband_d[j])
            for s in range(8):
                prep("lin%d_%d" % (j, s), lin_s[j, s], lin_d[j, s])
            for s in range(4):
                prep("lout%d_%d" % (j, s), lout_s[j, s], lout_d[j, s])
        else:
            for s in range(12):
                prep("rin%d_%d" % (j, s), rin_s[j, s], rin_d[j, s])
            for s in range(4):
                prep("rout%d_%d" % (j, s), rout_s[j, s], rout_d[j, s])
    for l in layers:
        if l % 2:
            continue
        j = l // 2
        lv, ld = lvec[j], lder[j]
        P.op(S_, lambda e, lv=lv, ld=ld: e.activation(out=ld[:, :, 3], in_=lv[:, :, 7], func=AF.Exp, scale=-1.0),
             reads=[b_lvec[j]], writes=[b_lder[j]])
        P.op(S_, lambda e, ld=ld: e.activation(out=ld[:, :, 2], in_=ld[:, :, 3], func=AF.Ln, bias=1.0),
             reads=[b_lder[j]], writes=[b_lder[j]])
        P.op(V, lambda e, ld=ld: e.tensor_scalar(out=ld[:, :, 2], in0=ld[:, :, 2], scalar1=-4.0, scalar2=None, op0=ALU.mult),
             reads=[b_lder[j]], writes=[b_lder[j]])
        P.op(V, lambda e, lv=lv, ld=ld: e.tensor_scalar(out=ld[:, :, 0:2], in0=lv[:, :, 5:7], scalar1=0.5, scalar2=None, op0=ALU.mult),
             reads=[b_lvec[j], b_lder[j]], writes=[b_lder[j]])
        P.op(V, lambda e, ld=ld: e.tensor_scalar(out=ld[:, :, 4], in0=ld[:, :, 2], scalar1=2.0, scalar2=None, op0=ALU.mult),
             reads=[b_lder[j]], writes=[b_lder[j]])
        P.op(V, lambda e, ld=ld: e.tensor_scalar(out=ld[:, :, 5], in0=ld[:, :, 2], scalar1=2.0, scalar2=float(math.log(0.25)), op0=ALU.mult, op1=ALU.add),
             reads=[b_lder[j]], writes=[b_lder[j]])

    def pre_chain(j):
        bs = b_stat[j]
        hb, bhb = hn2[j % 2], b_hn2[j % 2]
        P.op(S_, lambda e: e.activation(out=junk[:], in_=xt[:, j, :], func=AF.Square, scale=1.0 / 32.0, accum_out=stat[:, j:j + 1]),
             reads=[b_xt[j]], writes=[b_junk, bs])
        P.op(G, lambda e: e.tensor_scalar(out=stat[:, 8 + j:9 + j], in0=stat[:, j:j + 1], scalar1=RMS_EPS, scalar2=None, op0=ALU.add),
             reads=[bs], writes=[b_stat[8 + j]])
        P.op(G, lambda e: e.tensor_tensor(out=stat[:, 8 + j:9 + j], in0=stat[:, 8 + j:9 + j], in1=cpow[:, 0:1], op=ALU.pow),
             reads=[b_stat[8 + j], b_cpow], writes=[b_stat[8 + j]])
        P.op(V, lambda e: e.scalar_tensor_tensor(out=hb[:], in0=xt[:, j, :], scalar=stat[:, 8 + j:9 + j], in1=gpre[:],
                                                 op0=ALU.mult, op1=ALU.mult),
             reads=[b_xt[j], b_stat[8 + j], b_gpre], writes=[bhb])

    def pre_T(j):
        hb, bhb = hn2[j % 2], b_hn2[j % 2]

        def tr(e):
            ins = None
            for kc in range(8):
                ins = e.transpose(out=ptb[:, kc * 128:(kc + 1) * 128], in_=hb[:, kc * 128:(kc + 1) * 128], identity=ident[:])
            return ins
        P.op(PE, tr, reads=[bhb, b_ident], writes=b_ptbh)
        P.op(S_, lambda e: e.activation(out=hT[:, :, j * 128:(j + 1) * 128],
                                        in_=ptb[:].rearrange("p (a b) -> p a b", b=128), func=AF.Copy),
             reads=b_ptbh, writes=[b_hT])

    def proj_fm(pool, w, bw, col0):
        pt, bp = pool.next()

        def mm(e):
            ins = None
            W = w.rearrange("p (k c) -> p k c", k=8)
            for kc in range(8):
                ins = e.matmul(pt[:], lhsT=W[:, kc, col0:col0 + 128], rhs=hT[:, kc, :], start=(kc == 0), stop=(kc == 7))
            return ins
        P.op(PE, mm, reads=[bw, b_hT], writes=[bp])
        return pt, bp

    def outproj(l, nkc, wname, kper, eps, nxt):
        P.tag = "L%d:out" % l
        ssA, ssB, rs = 16, 24, 32
        pool = Rot([5, 6])
        for half in range(2):
            ws = [wload("%s_%d" % (wname, half * 2 + kg), (lout_s if wname.startswith("lout") else rout_s)[int(wname[-1]), half * 2 + kg],
                        kper * 512) for kg in range(2)]
            for j in range(NSUB):
                po, bpo = pool.next()

                def mm(e, j=j, po=po, ws=ws):
                    ins = None
                    for kc in range(nkc):
                        W = ws[kc // kper][0].rearrange("p (k c) -> p k c", c=512)
                        ins = e.matmul(po[:], lhsT=yT[:, kc, j * 128:(j + 1) * 128], rhs=W[:, kc % kper, :],
                                       start=(kc == 0), stop=(kc == nkc - 1))
                    return ins
                P.op(PE, mm, reads=[ws[0][1], ws[1][1]] + b_yT[:nkc], writes=[bpo])
                if half == 0:
                    P.op(S_, lambda e, j=j, po=po: e.activation(out=otmp[:, j, :], in_=po[:], func=AF.Copy),
                         reads=[bpo], writes=[b_otmp[j]])
                    P.op(S_, lambda e, j=j, po=po: e.activation(out=junk[:, 0:512], in_=po[:], func=AF.Square, scale=1.0 / 32.0,
                                                                accum_out=stat[:, ssA + j:ssA + j + 1]),
                         reads=[bpo], writes=[b_junk, b_stat[ssA + j]])
                else:
                    P.op(S_, lambda e, j=j, po=po: e.activation(out=junk[:, 0:512], in_=po[:], func=AF.Square, scale=1.0 / 32.0,
                                                                accum_out=stat[:, ssB + j:ssB + j + 1]),
                         reads=[bpo], writes=[b_junk, b_stat[ssB + j]])
                    P.op(V, lambda e, j=j: e.scalar_tensor_tensor(out=stat[:, rs + j:rs + j + 1], in0=stat[:, ssA + j:ssA + j + 1], scalar=eps,
                                                                  in1=stat[:, ssB + j:ssB + j + 1], op0=ALU.add, op1=ALU.add),
                         reads=[b_stat[ssA + j], b_stat[ssB + j]], writes=[b_stat[rs + j]])
                    P.op(G, lambda e, j=j: e.tensor_tensor(out=stat[:, rs + j:rs + j + 1], in0=stat[:, rs + j:rs + j + 1], in1=cpow[:, 0:1], op=ALU.pow),
                         reads=[b_stat[rs + j], b_cpow], writes=[b_stat[rs + j]])
                    P.op(V, lambda e, j=j, po=po: e.scalar_tensor_tensor(out=po[:], in0=po[:], scalar=stat[:, rs + j:rs + j + 1],
                                                                         in1=gpost[:, 512:1024], op0=ALU.mult, op1=ALU.mult),
                         reads=[bpo, b_stat[rs + j], b_gpost], writes=[bpo])
                    P.op(V, lambda e, j=j, po=po: e.tensor_tensor(out=xt[:, j, 512:1024], in0=xt[:, j, 512:1024], in1=po[:], op=ALU.add),
                         reads=[bpo, b_xt[j]], writes=[b_xt[j]])
                    P.op(V, lambda e, j=j: e.scalar_tensor_tensor(out=otmp[:, j, :], in0=otmp[:, j, :], scalar=stat[:, rs + j:rs + j + 1],
                                                                  in1=gpost[:, 0:512], op0=ALU.mult, op1=ALU.mult),
                         reads=[b_otmp[j], b_stat[rs + j], b_gpost], writes=[b_otmp[j]])
                    P.op(V, lambda e, j=j: e.tensor_tensor(out=xt[:, j, 0:512], in0=xt[:, j, 0:512], in1=otmp[:, j, :], op=ALU.add),
                         reads=[b_otmp[j], b_xt[j]], writes=[b_xt[j]])
                    nxt.pre(j)
                    if j >= 1:
                        nxt.T(j - 1)
                    P.tag = "L%d:out" % l
        P.tag = "L%d:pre" % l
        nxt.T(NSUB - 1)

    def lru_layer(l, nxt):
        jj = l // 2
        lv, ld = lvec[jj], lder[jj]
        blv, bld = b_lvec[jj], b_lder[jj]
        dma(SY, "cst", wband[:], lband_s[jj].rearrange("p (g n) -> p g n", g=2), reads=[b_scr["lband%d" % jj]], writes=[b_wband])
        pool = Rot([0, 1, 2])
        gpool = Rot([3, 4])
        it = [0]

        def stageA(g):
            P.tag = "L%d:A" % l
            pb = g % 2
            xr, bxr, xcg, bxc, xb16, bx16 = xbraw[0], b_xbraw[0], xc[pb], b_xc[pb], xcbf[pb], b_xcbf[pb]
            w, bw = wload("lin%d_%d" % (jj, 2 * g), lin_s[jj, 2 * g], 3072)
            P.op(V, lambda e: e.tensor_copy(out=xr[:, :, 0:3], in_=halo[jj][:, 3 * g:3 * g + 3, :]),
                 reads=[b_halo[jj][g]], writes=[bxr])
            for ci in range(3):
                pt, bp = pool.next()

                def mm(e, pt=pt, ci=ci):
                    ins = None
                    W = w[:, 0:3072].rearrange("p (k c) -> p k c", k=8)
                    for kc in range(8):
                        ins = e.matmul(pt[:], lhsT=W[:, kc, ci * 128:(ci + 1) * 128], rhs=hT[:, kc, :], start=(kc == 0), stop=(kc == 7))
                    return ins
                P.op(PE, mm, reads=[bw, b_hT], writes=[bp])
                P.op(S_, lambda e, pt=pt, ci=ci: e.activation(out=xr[:, ci, 3:515], in_=pt[:], func=AF.Copy),
                     reads=[bp], writes=[bxr])
            P.op(V, lambda e: e.tensor_copy(out=halo[jj][:, 3 * g:3 * g + 3, :], in_=xr[:, :, 512:515]),
                 reads=[bxr], writes=[b_halo[jj][g]])
            for ci in range(3):
                c = 3 * g + ci
                P.op(S_, lambda e, ci=ci, c=c: e.activation(out=xcg[:, ci, :], in_=xr[:, ci, 0:512], func=AF.Identity,
                                                            scale=lv[:, c, 0:1], bias=lv[:, c, 4:5]),
                     reads=[bxr, blv], writes=[bxc])
                for k in range(1, 4):
                    P.op(V, lambda e, ci=ci, c=c, k=k: e.scalar_tensor_tensor(out=xcg[:, ci, :], in0=xr[:, ci, k:k + 512],
                                                                              scalar=lv[:, c, k:k + 1], in1=xcg[:, ci, :],
                                                                              op0=ALU.mult, op1=ALU.add),
                         reads=[bxr, blv, bxc], writes=[bxc])
            P.op(S_, lambda e: e.activation(out=xb16[:], in_=xcg[:], func=AF.Copy), reads=[bxc], writes=[bx16])

        def stageG(g):
            P.tag = "L%d:G" % l
            wg_, bwg = wload("lin%d_%d" % (jj, 2 * g + 1), lin_s[jj, 2 * g + 1], 3072)
            for co in range(3):
                tg_, btg = LT["tg"][co % 2]
                wgt, bwgt = LT["wg"][co]
                pgt, bpgt = pool.next()

                def mg(e, pgt=pgt, co=co):
                    ins = None
                    W = wg_[:, 0:3072].rearrange("p (k c) -> p k c", k=8)
                    for kc in range(8):
                        ins = e.matmul(pgt[:], lhsT=W[:, kc, co * 128:(co + 1) * 128], rhs=hT[:, kc, :], start=(kc == 0), stop=(kc == 7))
                    return ins
                P.op(PE, mg, reads=[bwg, b_hT], writes=[bpgt])
                P.op(S_, lambda e, pgt=pgt, tg_=tg_: e.activation(out=tg_[:], in_=pgt[:], func=AF.Tanh, scale=0.5), reads=[bpgt], writes=[btg])
                P.op(V, lambda e, pgt=pgt, tg_=tg_, wgt=wgt: e.scalar_tensor_tensor(out=wgt[:], in0=tg_[:], scalar=1.0, in1=pgt[:], op0=ALU.add, op1=ALU.mult),
                     reads=[btg, bpgt], writes=[bwgt])

        def stageB(g):
            P.tag = "L%d:B" % l
            pb = g % 2
            xb16, bx16 = xcbf[pb], b_xcbf[pb]
            for co in range(3):
                c = 3 * g + co
                sl = (3 * g + co) % NB
                tr_, btr = LT["tr"][co % 2]; ti_, bti = LT["ti"][sl]; a_, ba = LT["a"][sl]; m_, bm = LT["m"][sl]
                pa, bpa = gpool.next()
                px, bpx = gpool.next()
                for gi, (pg, bpg) in enumerate(((pa, bpa), (px, bpx))):
                    def gm(e, pg=pg, gi=gi, co=co):
                        ins = None
                        blks = [(bi, ci) for bi, (ci, co2) in enumerate(BL) if co2 == co]
                        for n, (bi, ci) in enumerate(blks):
                            o = (g * 7 + bi) * 128
                            ins = e.matmul(pg[:], lhsT=wband[:, gi, o:o + 128], rhs=xb16[:, ci, :], start=(n == 0), stop=(n == len(blks) - 1))
                        return ins
                    P.op(PE, gm, reads=[b_wband, bx16], writes=[bpg])
                P.op(S_, lambda e, pa=pa, tr_=tr_, c=c: e.activation(out=tr_[:], in_=pa[:], func=AF.Tanh, scale=0.5, bias=ld[:, c, 0:1]),
                     reads=[bpa, bld], writes=[btr])
                P.op(S_, lambda e, px=px, ti_=ti_, c=c: e.activation(out=ti_[:], in_=px[:], func=AF.Tanh, scale=0.5, bias=ld[:, c, 1:2]),
                     reads=[bpx, bld], writes=[bti])
                P.op(S_, lambda e, tr_=tr_, a_=a_, c=c: e.activation(out=a_[:], in_=tr_[:], func=AF.Exp, scale=ld[:, c, 2:3], bias=ld[:, c, 2:3]),
                     reads=[btr, bld], writes=[ba])
                P.op(S_, lambda e, tr_=tr_, m_=m_, c=c: e.activation(out=m_[:], in_=tr_[:], func=AF.Exp, scale=ld[:, c, 4:5], bias=ld[:, c, 5:6]),
                     reads=[btr, bld], writes=[bm])
                P.op(V, lambda e, m_=m_: e.tensor_scalar(out=m_[:], in0=m_[:], scalar1=QCLAMP, scalar2=None, op0=ALU.min),
                     reads=[bm], writes=[bm])

        def stageC(g):
            P.tag = "L%d:C" % l
            for co in range(3):
                m_, bm = LT["m"][(3 * g + co) % NB]
                P.op(S_, lambda e, m_=m_: e.activation(out=m_[:], in_=m_[:], func=AF.Sqrt, scale=-1.0, bias=0.25), reads=[bm], writes=[bm])

        def stageD(g):
            P.tag = "L%d:D" % l
            pb = g % 2
            xcg, bxc = xc[pb], b_xc[pb]
            for co in range(3):
                c = 3 * g + co
                tb = it[0] % 2
                it[0] += 1
                sl = (3 * g + co) % NB
                ti_, bti = LT["ti"][sl]; a_, ba = LT["a"][sl]; m_, bm = LT["m"][sl]
                u_, bu = LT["u"][tb]; hs_, bhs = LT["hs"][tb]; wgt, bwgt = LT["wg"][co]
                P.op(V, lambda e, ti_=ti_, u_=u_, co=co: e.scalar_tensor_tensor(out=u_[:], in0=ti_[:], scalar=1.0, in1=xcg[:, co, :],
                                                                               op0=ALU.add, op1=ALU.mult),
                     reads=[bti, bxc], writes=[bu])
                P.op(V, lambda e, u_=u_, m_=m_: e.tensor_tensor(out=u_[:], in0=u_[:], in1=m_[:], op=ALU.mult), reads=[bu, bm], writes=[bu])
                P.op(V, lambda e, a_=a_, u_=u_, hs_=hs_, c=c: e.tensor_tensor_scan(out=hs_[:], data0=a_[:], data1=u_[:], initial=hst[jj][:, c:c + 1],
                                                                                  op0=ALU.mult, op1=ALU.add),
                     reads=[ba, bu, b_hst[jj][c]], writes=[bhs])
                P.op(V, lambda e, hs_=hs_, c=c: e.tensor_copy(out=hst[jj][:, c:c + 1], in_=hs_[:, 511:512]), reads=[bhs], writes=[b_hst[jj][c]])
                P.op(V, lambda e, hs_=hs_, wgt=wgt, c=c: e.scalar_tensor_tensor(out=yT[:, c, :], in0=hs_[:], scalar=0.5, in1=wgt[:], op0=ALU.mult, op1=ALU.mult),
                     reads=[bhs, bwgt], writes=[b_yT[c]])

        stageA(0)
        for g in range(4):
            stageG(g)
            stageB(g)
            if g + 1 < 4:
                stageA(g + 1)
            stageC(g)
            stageD(g)
        outproj(l, 12, "lout%d" % jj, 6, RMS_EPS, nxt)

    def ret_layer(l, t, nxt):
        jj = l // 2
        S3 = S32[jj]
        bS3 = b_S32[jj]
        dma(SY, "cst", cosb[:], cos_d[:, t * T:(t + 1) * T], writes=[b_cos])
        dma(SY, "cst", sinb[:], sin_d[:, t * T:(t + 1) * T], writes=[b_sin])
        for k in range(8):
            P.op(S_, lambda e, k=k: e.activation(out=Sbf[:, k, :], in_=S3[:, k, :], func=AF.Copy), reads=[bS3[k]], writes=[b_Sbf[k]])
        pool = Rot([0, 1, 2, 3])
        def do_pair(pr):
            P.tag = "L%d:qkv" % l

            def rot_unit(dst, bdst, w, bw, hh):
                p1, bp1 = proj_fm(pool, w, bw, (2 * hh) * 128)
                p2, bp2 = proj_fm(pool, w, bw, (2 * hh + 1) * 128)
                t1, t2, t3, t4 = rt
                bt1, bt2, bt3, bt4 = b_rt
                P.op(V, lambda e: e.tensor_tensor(out=t1[:], in0=p1[:], in1=cosb[:], op=ALU.mult), reads=[bp1, b_cos], writes=[bt1])
                P.op(V, lambda e: e.tensor_tensor(out=t2[:], in0=p2[:], in1=sinb[:], op=ALU.mult), reads=[bp2, b_sin], writes=[bt2])
                P.op(V, lambda e: e.tensor_tensor(out=t3[:], in0=p2[:], in1=cosb[:], op=ALU.mult), reads=[bp2, b_cos], writes=[bt3])
                P.op(V, lambda e: e.tensor_tensor(out=t4[:], in0=p1[:], in1=sinb[:], op=ALU.mult), reads=[bp1, b_sin], writes=[bt4])
                P.op(V, lambda e: e.tensor_tensor(out=dst[:, 2 * hh, :], in0=t1[:], in1=t2[:], op=ALU.subtract),
                     reads=[bt1, bt2], writes=[bdst])
                P.op(V, lambda e: e.tensor_tensor(out=dst[:, 2 * hh + 1, :], in0=t3[:], in1=t4[:], op=ALU.add),
                     reads=[bt3, bt4], writes=[bdst])

            def v_piece(w, bw, hh, j):
                pv, bpv = pool.next()

                def mv(e):
                    ins = None
                    W = w.rearrange("p (k c) -> p k c", k=8)
                    for kc in range(8):
                        ins = e.matmul(pv[:], lhsT=hT[:, kc, j * 128:(j + 1) * 128], rhs=W[:, kc, :], start=(kc == 0), stop=(kc == 7))
                    return ins
                P.op(PE, mv, reads=[bw, b_hT], writes=[bpv])
                P.op(S_, lambda e: e.activation(out=vtok[:, j, hh * 512:(hh + 1) * 512], in_=pv[:], func=AF.Copy),
                     reads=[bpv], writes=[b_vtok])

            for qk, (dst, bdst) in enumerate(((qT, b_qT), (kT, b_kT))):
                w, bw = wload("rin%d_%d" % (jj, 2 * qk + pr), rin_s[jj, 2 * qk + pr], 4096)
                hv = 2 * pr + qk
                wv, bwv = wload("rin%d_%d" % (jj, 4 + hv), rin_s[jj, 4 + hv], 4096)
                for hh in range(2):
                    rot_unit(dst, bdst, w, bw, hh)
                    v_piece(wv, bwv, qk, 2 * hh)
                    v_piece(wv, bwv, qk, 2 * hh + 1)

            def gate_piece(s, mc):
                w, bw = gate_w[s]
                pg, bpg = proj_fm(gpool_r, w, bw, mc * 128)
                tb = mc % 2
                P.op(S_, lambda e: e.activation(out=rtg[tb][:], in_=pg[:], func=AF.Tanh, scale=0.5), reads=[bpg], writes=[b_rtg[tb]])
                P.op(V, lambda e: e.scalar_tensor_tensor(out=sgate[:, s * 4 + mc, :], in0=rtg[tb][:], scalar=1.0, in1=pg[:],
                                                         op0=ALU.add, op1=ALU.mult),
                     reads=[b_rtg[tb], bpg], writes=[b_sgate])
            gate_w = {}
            gpool_r = Rot([3, 7])
            P.tag = "L%d:kT" % l
            for j in range(NSUB):
                hb = j % 2

                def tk(e, j=j, hb=hb):
                    ins = None
                    for q in range(4):
                        ins = e.transpose(out=ptb[:, q * 128:(q + 1) * 128], in_=kT[:, q, j * 128:(j + 1) * 128], identity=ident[:])
                    return ins
                P.op(PE, tk, reads=[b_kT, b_ident], writes=b_ptbh)
                P.op(V, lambda e, j=j, pr=pr, hb=hb: e.tensor_tensor(out=ktok[:, j, :], in0=ptb[:, 0:512], in1=kdecb[:, pr * 512:(pr + 1) * 512], op=ALU.mult),
                     reads=b_ptbh + [b_rcon], writes=[b_ktok])
            P.tag = "L%d:chunk" % l
            zpool = Rot([0, 1, 2])
            spool = Rot([4, 6])
            sp, bsp = pf[5], b_pf[5]
            zb = {}

            def P1(j):
                pb = j % 2

                def ms(e):
                    ins = None
                    for hh in range(2):
                        for hf in range(2):
                            ins = e.matmul(sp[:, hh * 128:(hh + 1) * 128], lhsT=kT[:, 2 * hh + hf, j * 128:(j + 1) * 128],
                                           rhs=qT[:, 2 * hh + hf, j * 128:(j + 1) * 128], start=(hf == 0), stop=(hf == 1))
                    return ins
                P.op(PE, ms, reads=[b_kT, b_qT], writes=[bsp])
                P.op(V, lambda e: e.tensor_tensor(out=PT[pb][:].rearrange("p a b -> p (a b)"), in0=sp[:, 0:256],
                                                  in1=maskT[:, pr * 256:(pr + 1) * 256], op=ALU.mult),
                     reads=[bsp, b_rcon], writes=[b_PT[pb]])

            def P2(j):
                pb = j % 2
                for hh in range(2):
                    h = 2 * pr + hh
                    pz, bpz = zpool.next()
                    zb[(j, hh)] = (pz, bpz)

                    def mz(e, pz=pz, hh=hh, h=h):
                        e.matmul(pz[:], lhsT=PT[pb][:, hh, :], rhs=vtok[:, j, hh * 512:(hh + 1) * 512], start=True, stop=False)
                        e.matmul(pz[:], lhsT=qT[:, 2 * hh, j * 128:(j + 1) * 128], rhs=Sbf[:, 2 * h, :], start=False, stop=False)
                        return e.matmul(pz[:], lhsT=qT[:, 2 * hh + 1, j * 128:(j + 1) * 128], rhs=Sbf[:, 2 * h + 1, :], start=False, stop=True)
                    P.op(PE, mz, reads=[b_PT[pb], b_vtok, b_qT, b_Sbf[2 * h], b_Sbf[2 * h + 1]], writes=[bpz])
                for hh in range(2):
                    h = 2 * pr + hh
                    for hf in range(2):
                        k = 2 * h + hf
                        pst, bpst = spool.next()
                        P.op(PE, lambda e, pst=pst, hh=hh, hf=hf: e.matmul(pst[:], lhsT=ktok[:, j, hh * 256 + hf * 128: hh * 256 + (hf + 1) * 128],
                                                                          rhs=vtok[:, j, hh * 512:(hh + 1) * 512], start=True, stop=True),
                             reads=[b_ktok, b_vtok], writes=[bpst])
                        P.op(V, lambda e, pst=pst, k=k, h=h: e.scalar_tensor_tensor(out=S3[:, k, :], in0=S3[:, k, :], scalar=float(GAM[h] ** 128), in1=pst[:],
                                                                                  op0=ALU.mult, op1=ALU.add),
                             reads=[bpst, bS3[k]], writes=[bS3[k]])
                        P.op(S_, lambda e, k=k: e.activation(out=Sbf[:, k, :], in_=S3[:, k, :], func=AF.Copy), reads=[bS3[k]], writes=[b_Sbf[k]])
                for hh in range(2):
                    pz, bpz = zb[(j, hh)]
                    o6 = 40 + hh * 6
                    omv = 52 + hh * 2
                    P.op(V, lambda e, pz=pz, o6=o6: e.bn_stats(out=stat[:, o6:o6 + 6], in_=pz[:]), reads=[bpz], writes=[b_stat[o6]])
                    P.op(V, lambda e, o6=o6, omv=omv: e.bn_aggr(out=stat[:, omv:omv + 2], in_=stat[:, o6:o6 + 6]), reads=[b_stat[o6]], writes=[b_stat[omv]])

            def P3(j):
                pb = j
                mvv = stat[:, 52:56].rearrange("p (h t) -> p h t", t=2)
                P.op(G, lambda e: e.tensor_tensor(out=stat[:, 56:58], in0=mvv[:, :, 1], in1=epsc[:, 2 * pr:2 * pr + 2], op=ALU.add),
                     reads=[b_stat[52], b_stat[54], b_rcon], writes=[b_stat[56]])
                P.op(G, lambda e: e.tensor_tensor(out=stat[:, 56:58], in0=stat[:, 56:58], in1=cpow[:, 0:2], op=ALU.pow),
                     reads=[b_stat[56], b_cpow], writes=[b_stat[56]])
                P.op(V, lambda e: e.scalar_tensor_tensor(out=stat[:, 58:60], in0=mvv[:, :, 0], scalar=-1.0, in1=stat[:, 56:58], op0=ALU.mult, op1=ALU.mult),
                     reads=[b_stat[52], b_stat[54], b_stat[56]], writes=[b_stat[58]])
                for hh in range(2):
                    pz, bpz = zb[(j, hh)]
                    P.op(S_, lambda e, pz=pz, hh=hh: e.activation(out=gntok[pb][:, hh * 512:(hh + 1) * 512], in_=pz[:], func=AF.Identity,
                                                                  scale=stat[:, 56 + hh:57 + hh], bias=stat[:, 58 + hh:59 + hh]),
                         reads=[bpz, b_stat[56], b_stat[58]], writes=[b_gntok[pb]])

            def P4(j):
                pb = j

                tbk, btbk = (ptb, b_ptbh) if j % 2 == 0 else (pf3_bf, [b_pf[3]])

                def tg(e):
                    ins = None
                    for q in range(8):
                        ins = e.transpose(out=tbk[:, q * 128:(q + 1) * 128], in_=gntok[pb][:, q * 128:(q + 1) * 128], identity=ident[:])
                    return ins
                P.op(PE, tg, reads=[b_gntok[pb], b_ident], writes=btbk)
                P.op(V, lambda e: e.tensor_tensor(out=yT[:, pr * 8:(pr + 1) * 8, j * 128:(j + 1) * 128],
                                                  in0=tbk[:].rearrange("p (a b) -> p a b", b=128),
                                                  in1=sgate[:, :, j * 128:(j + 1) * 128], op=ALU.mult),
                     reads=btbk + [b_sgate], writes=b_yT[pr * 8:(pr + 1) * 8])

            P1(0)
            for j in range(NSUB):
                P2(j)
                if j + 1 < NSUB:
                    P1(j + 1)
                P.tag = "L%d:gate" % l
                s_ = j // 2
                if j % 2 == 0:
                    gate_w[s_] = wload("rin%d_%d" % (jj, 8 + 2 * pr + s_), rin_s[jj, 8 + 2 * pr + s_], 4096)
                gate_piece(s_, 2 * (j % 2))
                gate_piece(s_, 2 * (j % 2) + 1)
                P.tag = "L%d:chunk" % l
                P3(j)
            for j in range(NSUB):
                P4(j)
        for pr_ in range(2):
            do_pair(pr_)
        outproj(l, 16, "rout%d" % jj, 8, RMS_EPS / 4.0, nxt)

    steps = [(sq, t, l) for sq in range(NSEQ) for t in range(NT) for l in layers]

    def xrows(sq, t, j):
        r0 = sq * SEQ + t * T + j * 128
        return r0, r0 + 128

    class Nxt:
        def __init__(self, k):
            self.k = k
            self.cur = steps[k]
            self.nx = steps[k + 1] if k + 1 < len(steps) else None

        def pre(self, j):
            P.tag = "L%d:pre" % self.cur[2]
            sq, t, l = self.cur
            if l == layers[-1]:
                a, b = xrows(sq, t, j)
                dma(SY, "st", out_d[a:b, :], xt[:, j, :], reads=[b_xt[j]])
                if self.nx is not None:
                    a, b = xrows(self.nx[0], self.nx[1], j)
                    dma(SY, "xl", xt[:, j, :], x_d[a:b, :], writes=[b_xt[j]])
            if self.nx is not None:
                if j == 0:
                    dma(SY, "cst", gpre[:], gpre_d[self.nx[2]], writes=[b_gpre])
                pre_chain(j)

        def T(self, j):
            if self.nx is not None:
                pre_T(j)

    for j in range(NSUB):
        a, b = xrows(0, 0, j)
        dma(SY, "xl", xt[:, j, :], x_d[a:b, :], writes=[b_xt[j]])
    dma(SY, "cst", gpre[:], gpre_d[layers[0]], writes=[b_gpre])
    for j in range(NSUB):
        pre_chain(j)
        pre_T(j)
    for k, (sq, t, l) in enumerate(steps):
        jj = l // 2
        if t == 0:
            if l % 2 == 0:
                P.op(V, lambda e, jj=jj: e.memset(hst[jj][:], 0.0), writes=b_hst[jj])
                P.op(V, lambda e, jj=jj: e.memset(halo[jj][:], 0.0), writes=b_halo[jj])
            else:
                P.op(G, lambda e, jj=jj: e.memset(S32[jj][:], 0.0), writes=b_S32[jj])
        dma(SY, "cst", gpost[:], gpost_d[l], writes=[b_gpost])
        P.tag = "L%d" % l
        if l % 2 == 0:
            lru_layer(l, Nxt(k))
        else:
            ret_layer(l, t, Nxt(k))
    P.wait_all(SY, ["st"])

    P.finalize(reorder=not os.environ.get("K_NO_REORDER"))
    sems = {}
    for k in P.cnt:
        sems[k] = es.enter_context(nc.semaphore("s_" + k.replace(".", "_")))
    with nc.Block() as block:
        def emit(eng_name):
            def body(e):
                for deps, fn, key, inc in P.q[eng_name]:
                    for k, v in deps:
                        e.wait_ge(sems[k], v)
                    if fn is not None:
                        ins = fn(e)
                        ins.then_inc(sems[key], inc)
            return body
        if os.environ.get("KDUMP_TAGS"):
            class Cnt:
                def __init__(self):
                    self.n = 0
                def matmul(self, *a, **k):
                    self.n += 1
                    return self
                transpose = matmul
            rows = []
            for (deps, fn, key, inc), tg in zip(P.q["tensor"], P.tags["tensor"]):
                if fn is None:
                    continue
                c = Cnt()
                fn(c)
                rows.append((tg, c.n))
            import json
            json.dump(rows, open(os.environ["KDUMP_TAGS"], "w"))
        block.tensor(emit("tensor"))
        block.vector(emit("vector"))
        block.scalar(emit("scalar"))
        block.gpsimd(emit("gpsimd"))
        block.sync(emit("sync"))
    es.close()
    return nc


def _consts(SEQ):
    pos = np.arange(SEQ, dtype=np.float32)
    inv_freq = (10000.0 ** (-np.arange(0, 256, 2, dtype=np.float32) / 256)).astype(np.float32)
    ang = (pos[:, None] * inv_freq[None, :]).astype(np.float32)
    cosT = np.ascontiguousarray(np.cos(ang).T.astype(np.float32))
    sinT = np.ascontiguousarray(np.sin(ang).T.astype(np.float32))
    idx = np.arange(128, dtype=np.float64)
    rcon = np.zeros((128, 512 + 1024 + 4), np.float32)
    for h in range(4):
        g = GAM[h]
        m = (g ** (-(idx[:, None] + 1.0))) * (idx[None, :] >= idx[:, None]) * (256 ** -0.5)
        rcon[:, h * 128:(h + 1) * 128] = m
        rcon[:, 512 + h * 256: 512 + (h + 1) * 256] = ((g ** (127.0 - idx)) * (256 ** -0.5))[:, None]
        rcon[:, 1536 + h] = GN_EPS * g ** (-2.0 * (idx + 1.0))
    ident = np.eye(128, dtype=np.float32).astype(ml_dtypes.bfloat16)
    return cosT, sinT, rcon, ident


def _layout_weights(inp):
    f = np.float32
    lw_in = np.asarray(inp["lru_w_in"], f)
    lin = np.zeros((2, 8, 128, 3072), f)
    for j in range(2):
        w = lw_in[j].reshape(8, 128, 3072)
        for g in range(4):
            for br in range(2):
                cols = w[:, :, br * 1536 + g * 384: br * 1536 + (g + 1) * 384]
                lin[j, 2 * g + br] = cols.transpose(1, 0, 2).reshape(128, 3072)
    lw_out = np.asarray(inp["lru_w_out"], f)
    lout = np.zeros((2, 4, 128, 3072), f)
    for j in range(2):
        w = lw_out[j].reshape(2, 6, 128, 2, 512)
        for half in range(2):
            for kg in range(2):
                lout[j, half * 2 + kg] = w[kg, :, :, half, :].transpose(1, 0, 2).reshape(128, 3072)
    lband = np.zeros((2, 128, 2, 4, 7, 128), f)
    for j in range(2):
        for gi, nm in enumerate(("lru_w_a", "lru_w_x")):
            wa = np.asarray(inp[nm], f)[j]
            for g in range(4):
                full = np.zeros((384, 384), f)
                for n in range(4):
                    full[96 * n:96 * n + 96, 96 * n:96 * n + 96] = wa[4 * g + n]
                for bi, (ci, co) in enumerate(BL):
                    lband[j, :, gi, g, bi, :] = full[ci * 128:(ci + 1) * 128, co * 128:(co + 1) * 128]
    lband = lband.reshape(2, 128, 2 * 3584)
    lvec = np.zeros((2, 128, 12, 9), f)
    for j in range(2):
        cw = np.asarray(inp["lru_conv_w"], f)[j]
        for k in range(4):
            lvec[j, :, :, k] = cw[k].reshape(12, 128).T
        for k, nm in enumerate(("lru_conv_b", "lru_b_a", "lru_b_x", "lru_lambda")):
            lvec[j, :, :, 4 + k] = np.asarray(inp[nm], f)[j].reshape(12, 128).T
    lvec = lvec.reshape(2, 128, 108)
    rw_in = np.asarray(inp["ret_w_in"], f)
    rin = np.zeros((2, 12, 128, 4096), f)
    for j in range(2):
        w = rw_in[j].reshape(8, 128, 12, 512)
        rin[j] = w.transpose(2, 1, 0, 3).reshape(12, 128, 4096)
    rw_out = np.asarray(inp["ret_w_out"], f)
    rout = np.zeros((2, 4, 128, 4096), f)
    for j in range(2):
        w = rw_out[j].reshape(2, 8, 128, 2, 512)
        for half in range(2):
            for kg in range(2):
                rout[j, half * 2 + kg] = w[kg, :, :, half, :].transpose(1, 0, 2).reshape(128, 4096)
    gpre = np.ascontiguousarray(np.broadcast_to(np.asarray(inp["norm_pre"], f)[:, None, :], (4, 128, D)))
    gpost = np.ascontiguousarray(np.broadcast_to(np.asarray(inp["norm_post"], f)[:, None, :], (4, 128, D)))
    return dict(gpre=gpre, gpost=gpost, lru_in=lin, lru_out=lout, lru_band=lband, lru_vec=lvec, ret_in=rin, ret_out=rout)


def run(inputs, ncores=NCORE, layers=(0, 1, 2, 3)):
    x = np.asarray(inputs["x"], np.float32)
    B, SEQ, _ = x.shape
    nseq = B // ncores
    nc = build(nseq, SEQ, list(layers))
    shared = _layout_weights(inputs)
    cosT, sinT, rcon, ident = _consts(SEQ)
    shared.update(cosT=cosT, sinT=sinT, rcon=rcon, ident=ident)
    in_maps = []
    for c in range(ncores):
        m = dict(shared)
        m["x"] = np.ascontiguousarray(x[c * nseq:(c + 1) * nseq].reshape(nseq * SEQ, D))
        in_maps.append(m)
    res = run_bass_kernel_spmd(nc, in_maps, core_ids=list(range(ncores)))
    out = np.stack([r["out"].reshape(nseq, SEQ, D) for r in res.results], axis=0).reshape(B, SEQ, D)
    return out.astype(np.float32)


def kernel(**inputs):
    return run(inputs)
```

```python
from contextlib import ExitStack
import math
import os
import numpy as np
import ml_dtypes
import concourse.bass as bass
import concourse.mybir as mybir
from concourse.bass_utils import run_bass_kernel_spmd

F32 = mybir.dt.float32
BF16 = mybir.dt.bfloat16
ALU = mybir.AluOpType
AF = mybir.ActivationFunctionType

D = 1024
DR = 1536
T = 512
NSUB = 4
NCORE = 8
RMS_EPS = 1e-6
GN_EPS = 1e-5
BL = [(0, 0), (0, 1), (1, 0), (1, 1), (1, 2), (2, 1), (2, 2)]
GAM = [1.0 - 2.0 ** (-5.0 - h) for h in range(4)]
QCLAMP = float(np.nextafter(np.float32(0.25), np.float32(0.0)))


class Buf:
    __slots__ = ("name", "arena", "lo", "hi", "w", "r", "ov")

    def __init__(self, name, arena=None, lo=0, hi=0):
        self.name, self.arena, self.lo, self.hi = name, arena, lo, hi
        self.w = None
        self.r = set()
        self.ov = [self]


class _Dry:
    def __init__(self, eng):
        self.eng, self.cost, self.tbl = eng, 0.0, None

    @staticmethod
    def _n(ap):
        sh = ap.shape
        n = 1
        for d in sh[1:]:
            n *= int(d)
        return n

    def then_inc(self, *a, **k):
        return self

    def __getattr__(self, name):
        def f(*a, **k):
            if name == "matmul":
                self.cost += max(self._n(k["rhs"]), 64) / 2400.0 * 1.2
            elif name == "transpose":
                self.cost += 0.075
            elif name == "dma_start":
                o = k["out"]
                nb = self._n(o) * int(o.shape[0]) * (2 if o.dtype == BF16 else 4)
                self.cost += 2.0 + nb / 150e3
            elif name == "activation":
                self.cost += (self._n(k["out"]) + 230) / 1200.0 + (0.1 if k.get("accum_out") is not None else 0.0)
                fnc = k["func"]
                if fnc in (AF.Exp, AF.Tanh):
                    self.tbl = "exp"
                elif fnc == AF.Sqrt:
                    self.tbl = "sqrt"
                elif fnc == AF.Ln:
                    self.tbl = "ln"
            else:
                o = k.get("out", a[0] if a else None)
                n = self._n(k["in_"]) if name == "bn_stats" else self._n(o)
                if self.eng == "gpsimd":
                    if name == "memset":
                        self.cost += 0.3 + n * 0.0003
                    else:
                        self.cost += 0.2 + n * 0.015 + (0.3 if k.get("op") == ALU.pow else 0.0)
                else:
                    self.cost += (n + 150) / 960.0
            return self
        return f


class Prog:
    ENG = ("tensor", "vector", "scalar", "gpsimd", "sync")

    def __init__(self):
        self.ops = []
        self.arena_bufs = {}
        self.tag = ""
        self.dma_k = {"cst": 8, "prep": 6}
        self.q = None
        self.cnt = None

    def buf(self, name, arena=None, lo=0, hi=0):
        b = Buf(name, arena, lo, hi)
        b.r = set()
        if arena is not None:
            lst = self.arena_bufs.setdefault(arena, [])
            for o in lst:
                if o.lo < hi and lo < o.hi:
                    o.ov.append(b)
                    b.ov.append(o)
            lst.append(b)
        return b

    def op(self, eng, fn, reads=(), writes=(), dma=None):
        idx = len(self.ops)
        preds = set()
        for b in reads:
            for o in b.ov:
                if o.w is not None:
                    preds.add(o.w)
        for b in writes:
            for o in b.ov:
                if o.w is not None:
                    preds.add(o.w)
                preds |= o.r
        preds.discard(idx)
        d = _Dry(eng)
        if fn is not None:
            fn(d)
        self.ops.append([eng, fn, dma, preds, d.cost, d.tbl, self.tag])
        for b in reads:
            b.r.add(idx)
        for b in writes:
            b.w = idx
            b.r = set()
        return idx

    def wait_all(self, eng, streams):
        preds = set(i for i, o in enumerate(self.ops) if o[2] in streams)
        self.ops.append([eng, None, None, preds, 0.0, None, "end"])

    def finalize(self, reorder=True):
        import heapq
        ops = self.ops
        n = len(ops)
        succ = [[] for _ in range(n)]
        indeg = [0] * n
        for i, o in enumerate(ops):
            indeg[i] = len(o[3])
            for p in o[3]:
                succ[p].append(i)
        order = []
        prio = list(range(n))
        if os.environ.get("K_PRIO", "bl") == "bl":
            bl = [0.0] * n
            for i in range(n - 1, -1, -1):
                m = 0.0
                for sct in succ[i]:
                    if bl[sct] > m:
                        m = bl[sct]
                bl[i] = m + ops[i][4] + 0.2
            rank = sorted(range(n), key=lambda i: (-bl[i], i))
            for r, i in enumerate(rank):
                prio[i] = r
        inv = [0] * n
        for i, p in enumerate(prio):
            inv[p] = i
        self.sched = []
        if reorder:
            fin = [0.0] * n
            efree = {e: 0.0 for e in self.ENG}
            etbl = {e: None for e in self.ENG}
            wait = {e: [] for e in self.ENG}
            avail = {e: [] for e in self.ENG}
            for i in range(n):
                if indeg[i] == 0:
                    heapq.heappush(wait[ops[i][0]], (0.0, prio[i]))
            done = 0
            while done < n:
                best = None
                for e in self.ENG:
                    w, a = wait[e], avail[e]
                    while w and w[0][0] <= efree[e]:
                        heapq.heappush(a, heapq.heappop(w)[1])
                    if a:
                        cand = (efree[e], a[0], e, True)
                    elif w:
                        cand = (w[0][0], w[0][1], e, False)
                    else:
                        continue
                    if best is None or cand[:2] < best[:2]:
                        best = cand
                st, pi, e, from_a = best
                i = inv[pi]
                if from_a:
                    if e == "scalar" and ops[i][5] is not None and ops[i][5] != etbl[e] and len(avail[e]) > 1:
                        small = heapq.nsmallest(6, avail[e])
                        alt = [pj for pj in small if ops[inv[pj]][5] is None or ops[inv[pj]][5] == etbl[e]]
                        if alt:
                            pi = alt[0]
                            i = inv[pi]
                            avail[e].remove(pi)
                            heapq.heapify(avail[e])
                        else:
                            heapq.heappop(avail[e])
                    else:
                        heapq.heappop(avail[e])
                else:
                    heapq.heappop(wait[e])
                o = ops[i]
                dur = o[4]
                if e == "scalar" and o[5] is not None and o[5] != etbl[e]:
                    dur += 1.3
                    etbl[e] = o[5]
                if o[2]:
                    efree[e] = st + 0.06
                    fin[i] = st + dur
                else:
                    efree[e] = st + dur + 0.06
                    fin[i] = st + dur + 0.15
                order.append(i)
                self.sched.append((i, e, st, fin[i]))
                done += 1
                for sct in succ[i]:
                    indeg[sct] -= 1
                    if indeg[sct] == 0:
                        rt = 0.0
                        for p in ops[sct][3]:
                            if fin[p] > rt:
                                rt = fin[p]
                        heapq.heappush(wait[ops[sct][0]], (rt, prio[sct]))
            self.est_us = max(fin) if fin else 0.0
        else:
            order = list(range(n))
        self.q = {e: [] for e in self.ENG}
        self.tags = {e: [] for e in self.ENG}
        cnt = {}
        seen = {e: {} for e in self.ENG}
        ev = [None] * n
        dma_n = {}
        dma_hist = {}
        for i in order:
            eng, fn, dma, preds, dur, tbl, tag = ops[i]
            deps = {}
            sn = seen[eng]
            for p in preds:
                k, v = ev[p]
                if eng == "tensor" and k == "tensor":
                    continue
                if sn.get(k, 0) >= v:
                    continue
                if deps.get(k, 0) < v:
                    deps[k] = v
            if fn is None:
                self.q[eng].append((tuple(deps.items()), None, None, 0))
                self.tags[eng].append(tag)
                ev[i] = (eng, cnt.get(eng, 0))
                continue
            if dma:
                m = dma_n.get(dma, 0)
                dma_n[dma] = m + 1
                K = self.dma_k.get(dma, 4)
                key, inc = "%s.%d" % (dma, m % K), 16
                if m >= K and sn.get(key, 0) < 16 * (m // K):
                    deps[key] = max(deps.get(key, 0), 16 * (m // K))
            else:
                key, inc = eng, 1
            for k, v in deps.items():
                sn[k] = v
            val = cnt.get(key, 0) + inc
            cnt[key] = val
            ev[i] = (key, val)
            self.q[eng].append((tuple(deps.items()), fn, key, inc))
            self.tags[eng].append(tag)
        self.cnt = cnt


def build(NSEQ, SEQ, layers, dbg_specs=None):
    NT = SEQ // T
    nc = bass.Bass("TRN2", target_bir_lowering=False)
    P = Prog()
    es = ExitStack()

    def din(name, shape, dt=F32):
        return nc.dram_tensor(name, list(shape), dt, kind="ExternalInput").ap()

    def dscr(name, shape, dt=BF16):
        return nc.dram_tensor(name, list(shape), dt, kind="Internal").ap()

    x_d = din("x", [NSEQ * SEQ, D])
    out_d = nc.dram_tensor("out", [NSEQ * SEQ, D], F32, kind="ExternalOutput").ap()
    gpre_d = din("gpre", [4, 128, D])
    gpost_d = din("gpost", [4, 128, D])
    lin_d = din("lru_in", [2, 8, 128, 3072])
    lout_d = din("lru_out", [2, 4, 128, 3072])
    lband_d = din("lru_band", [2, 128, 2 * 3584])
    lvec_d = din("lru_vec", [2, 128, 12 * 9])
    rin_d = din("ret_in", [2, 12, 128, 4096])
    rout_d = din("ret_out", [2, 4, 128, 4096])
    cos_d = din("cosT", [128, SEQ])
    sin_d = din("sinT", [128, SEQ])
    rcon_d = din("rcon", [128, 512 + 1024 + 4])
    ident_d = din("ident", [128, 128], BF16)

    lin_s = dscr("lin_s", [2, 8, 128, 3072])
    lout_s = dscr("lout_s", [2, 4, 128, 3072])
    lband_s = dscr("lband_s", [2, 128, 2 * 3584])
    rin_s = dscr("rin_s", [2, 12, 128, 4096])
    rout_s = dscr("rout_s", [2, 4, 128, 4096])

    def sb(name, shape, dt=F32):
        return es.enter_context(nc.sbuf_tensor("sb_" + name, list(shape), dt))

    def ps(name, shape, dt=F32):
        return es.enter_context(nc.psum_tensor("ps_" + name, list(shape), dt))

    xt = sb("xt", [128, NSUB, D]);            b_xt = [P.buf("xt%d" % j) for j in range(NSUB)]
    hn2 = [sb("hn%d" % i, [128, D], BF16) for i in range(2)]
    b_hn2 = [P.buf("hn%d" % i) for i in range(2)]
    junk = sb("junk", [128, D], BF16);        b_junk = P.buf("junk")
    hT = sb("hT", [128, 8, T], BF16);         b_hT = P.buf("hT")
    gpre = sb("gpre", [128, D]);              b_gpre = P.buf("gpre")
    gpost = sb("gpost", [128, D]);            b_gpost = P.buf("gpost")
    NW = 3
    wr = [sb("wr%d" % i, [128, 4096], BF16) for i in range(NW)]
    b_wr = [P.buf("wr%d" % i) for i in range(NW)]
    yT = sb("yT", [128, 16, T], BF16);        b_yT = [P.buf("yT%d" % c) for c in range(16)]
    otmp = sb("otmp", [128, NSUB, 512]);      b_otmp = [P.buf("otmp%d" % j) for j in range(NSUB)]
    ident = sb("ident", [128, 128], BF16);    b_ident = P.buf("ident")
    stat = sb("stat", [128, 64]);
    b_stat = [P.buf("stat%d" % i) for i in range(64)]
    S32 = [sb("S32_%d" % i, [128, 8, 512]) for i in range(2)]
    b_S32 = [[P.buf("S32_%d_%d" % (i, k)) for k in range(8)] for i in range(2)]
    lvec = [sb("lvec%d" % i, [128, 12, 9]) for i in range(2)]
    b_lvec = [P.buf("lvec%d" % i) for i in range(2)]
    lder = [sb("lder%d" % i, [128, 12, 6]) for i in range(2)]
    b_lder = [P.buf("lder%d" % i) for i in range(2)]
    hst = [sb("hst%d" % i, [128, 12]) for i in range(2)]
    b_hst = [[P.buf("hst%d_%d" % (i, c)) for c in range(12)] for i in range(2)]
    halo = [sb("halo%d" % i, [128, 12, 3]) for i in range(2)]
    b_halo = [[P.buf("halo%d_%d" % (i, g)) for g in range(4)] for i in range(2)]
    wband = sb("wband", [128, 2, 4 * 7 * 128], BF16); b_wband = P.buf("wband")
    rcon = sb("rcon", [128, 512 + 1024 + 4]);         b_rcon = P.buf("rcon")
    maskT = rcon[:, 0:512]
    kdecb = rcon[:, 512:1536]
    epsc = rcon[:, 1536:1540]

    cpow = sb("cpow", [128, 520]);                     b_cpow = P.buf("cpow")
    ARW = 16512
    arena = sb("arena", [128, ARW])

    class Scope:
        def __init__(self):
            self.off = 0

        def f32(self, name, shape):
            n = int(np.prod(shape))
            lo = self.off
            self.off += n
            assert self.off <= ARW, (name, self.off)
            ap = arena[:, lo:lo + n]
            if len(shape) == 2:
                ap = ap.rearrange("p (a b) -> p a b", b=shape[1])
            return ap, P.buf(name, "ar", lo, lo + n)

        def bf(self, name, shape):
            n = int(np.prod(shape))
            w = (n + 1) // 2
            lo = self.off
            self.off += w
            assert self.off <= ARW, (name, self.off)
            ap = arena[:, lo:lo + w].bitcast(BF16)
            if len(shape) == 2:
                ap = ap.rearrange("p (a b) -> p a b", b=shape[1])
            elif len(shape) == 3:
                ap = ap.rearrange("p (a b c) -> p a b c", b=shape[1], c=shape[2])
            return ap, P.buf(name, "ar", lo, lo + w)

    L = Scope()
    xbraw, b_xbraw = zip(*[L.f32("xbraw%d" % i, [3, 516]) for i in range(1)])
    xc, b_xc = zip(*[L.f32("xc%d" % i, [3, 512]) for i in range(2)])
    xcbf, b_xcbf = zip(*[L.bf("xcbf%d" % i, [3, 512]) for i in range(2)])
    LT = {}
    NB = 5
    for nm in ("tr", "ti", "a", "m", "tg", "wg"):
        LT[nm] = [L.f32("l_%s%d" % (nm, i), [512]) for i in range({"ti": NB, "a": NB, "m": 3, "wg": 3}.get(nm, 2))]
    for i in range(2):
        bx = P.buf("yx%d" % i)
        b_yT[12 + 2 * i] = b_yT[13 + 2 * i] = bx
        LT["m"].append((yT[:, 12 + 2 * i:14 + 2 * i, :].bitcast(F32).rearrange("p a b -> p (a b)"), bx))
    LT["u"] = [(otmp[:, i, :], b_otmp[i]) for i in range(2)]
    LT["hs"] = [(otmp[:, 2 + i, :], b_otmp[2 + i]) for i in range(2)]
    R = Scope()
    qT, b_qT = R.bf("qT", [4, 512])
    kT, b_kT = R.bf("kT", [4, 512])
    vtok, b_vtok = R.bf("vtok", [NSUB, 1024])
    ktok, b_ktok = R.bf("ktok", [NSUB, 512])
    sgate, b_sgate = R.bf("sgate", [8, 512])
    Sbf, b_Sbf_all = R.bf("Sbf", [8, 512])
    b_Sbf = [P.buf("Sbf%d" % k, "ar", b_Sbf_all.lo + k * 256, b_Sbf_all.lo + (k + 1) * 256) for k in range(8)]
    cosb, b_cos = R.f32("cos", [512])
    sinb, b_sin = R.f32("sin", [512])
    PT, b_PT = zip(*[R.bf("PT%d" % i, [2, 128]) for i in range(2)])
    gntok, b_gntok = zip(*[R.bf("gntok%d" % i, [1024]) for i in range(4)])
    rtg, b_rtg = zip(*[R.f32("rtg%d" % i, [512]) for i in range(2)])
    rt, b_rt = zip(*[R.f32("rt%d" % i, [512]) for i in range(4)])

    pf = [ps("pf%d" % i, [128, 512]) for i in range(7)]
    b_pf = [P.buf("pf%d" % i) for i in range(7)]
    ptb = ps("ptb", [128, 1024], BF16)
    b_ptbh = [P.buf("ptb")]
    pf.append(ptb[:].bitcast(F32))
    b_pf.append(b_ptbh[0])
    pf3_bf = pf[3][:].bitcast(BF16)

    class Rot:
        def __init__(self, idx):
            self.idx, self.i = idx, 0

        def next(self):
            k = self.idx[self.i % len(self.idx)]
            self.i += 1
            return pf[k], b_pf[k]

    V, S_, G, PE, SY = "vector", "scalar", "gpsimd", "tensor", "sync"

    def dma(eng, stream, out, in_, reads=(), writes=()):
        P.op(eng, lambda e: e.dma_start(out=out, in_=in_), reads=reads, writes=writes, dma=stream)

    b_scr = {}

    def prep(name, dst, src):
        b = P.buf(name)
        b_scr[name] = b
        dma(G, "prep", dst, src, writes=[b])

    wload_n = [0]

    def wload(name, src, width):
        i = wload_n[0] % NW
        wload_n[0] += 1
        dma(SY, "wl", wr[i][:, 0:width], src, reads=[b_scr[name]], writes=[b_wr[i]])
        return wr[i], b_wr[i]

    P.op(G, lambda e: e.memset(cpow[:, 0:8], -0.5), writes=[b_cpow])
    P.op(G, lambda e: e.memset(cpow[:, 8:520], 0.5), writes=[b_cpow])
    dma(G, "cstg", ident[:], ident_d, writes=[b_ident])
    dma(G, "cstg", rcon[:], rcon_d, writes=[b_rcon])
    for l in layers:
        j = l // 2
        if l % 2 == 0:
            dma(G, "cstg", lvec[j][:], lvec_d[j].rearrange("p (c k) -> p c k", k=9), writes=[b_lvec[j]])
    done_w = set()
    for l in layers:
        j = l // 2
        if (l % 2, j) in done_w:
            continue
        done_w.add((l % 2, j))
        if l % 2 == 0:
            prep("lband%d" % j, lband_s[j], lband_d[j])
            for s in range(8):
                prep("lin%d_%d" % (j, s), lin_s[j, s], lin_d[j, s])
            for s in range(4):
                prep("lout%d_%d" % (j, s), lout_s[j, s], lout_d[j, s])
        else:
            for s in range(12):
                prep("rin%d_%d" % (j, s), rin_s[j, s], rin_d[j, s])
            for s in range(4):
                prep("rout%d_%d" % (j, s), rout_s[j, s], rout_d[j, s])
    for l in layers:
        if l % 2:
            continue
        j = l // 2
        lv, ld = lvec[j], lder[j]
        P.op(S_, lambda e, lv=lv, ld=ld: e.activation(out=ld[:, :, 3], in_=lv[:, :, 7], func=AF.Exp, scale=-1.0),
             reads=[b_lvec[j]], writes=[b_lder[j]])
        P.op(S_, lambda e, ld=ld: e.activation(out=ld[:, :, 2], in_=ld[:, :, 3], func=AF.Ln, bias=1.0),
             reads=[b_lder[j]], writes=[b_lder[j]])
        P.op(V, lambda e, ld=ld: e.tensor_scalar(out=ld[:, :, 2], in0=ld[:, :, 2], scalar1=-4.0, scalar2=None, op0=ALU.mult),
             reads=[b_lder[j]], writes=[b_lder[j]])
        P.op(V, lambda e, lv=lv, ld=ld: e.tensor_scalar(out=ld[:, :, 0:2], in0=lv[:, :, 5:7], scalar1=0.5, scalar2=None, op0=ALU.mult),
             reads=[b_lvec[j], b_lder[j]], writes=[b_lder[j]])
        P.op(V, lambda e, ld=ld: e.tensor_scalar(out=ld[:, :, 4], in0=ld[:, :, 2], scalar1=2.0, scalar2=None, op0=ALU.mult),
             reads=[b_lder[j]], writes=[b_lder[j]])
        P.op(V, lambda e, ld=ld: e.tensor_scalar(out=ld[:, :, 5], in0=ld[:, :, 2], scalar1=2.0, scalar2=float(math.log(0.25)), op0=ALU.mult, op1=ALU.add),
             reads=[b_lder[j]], writes=[b_lder[j]])

    def pre_chain(j):
        bs = b_stat[j]
        hb, bhb = hn2[j % 2], b_hn2[j % 2]
        P.op(S_, lambda e: e.activation(out=junk[:], in_=xt[:, j, :], func=AF.Square, scale=1.0 / 32.0, accum_out=stat[:, j:j + 1]),
             reads=[b_xt[j]], writes=[b_junk, bs])
        P.op(G, lambda e: e.tensor_scalar(out=stat[:, 8 + j:9 + j], in0=stat[:, j:j + 1], scalar1=RMS_EPS, scalar2=None, op0=ALU.add),
             reads=[bs], writes=[b_stat[8 + j]])
        P.op(G, lambda e: e.tensor_tensor(out=stat[:, 8 + j:9 + j], in0=stat[:, 8 + j:9 + j], in1=cpow[:, 0:1], op=ALU.pow),
             reads=[b_stat[8 + j], b_cpow], writes=[b_stat[8 + j]])
        P.op(V, lambda e: e.scalar_tensor_tensor(out=hb[:], in0=xt[:, j, :], scalar=stat[:, 8 + j:9 + j], in1=gpre[:],
                                                 op0=ALU.mult, op1=ALU.mult),
             reads=[b_xt[j], b_stat[8 + j], b_gpre], writes=[bhb])

    def pre_T(j):
        hb, bhb = hn2[j % 2], b_hn2[j % 2]

        def tr(e):
            ins = None
            for kc in range(8):
                ins = e.transpose(out=ptb[:, kc * 128:(kc + 1) * 128], in_=hb[:, kc * 128:(kc + 1) * 128], identity=ident[:])
            return ins
        P.op(PE, tr, reads=[bhb, b_ident], writes=b_ptbh)
        P.op(S_, lambda e: e.activation(out=hT[:, :, j * 128:(j + 1) * 128],
                                        in_=ptb[:].rearrange("p (a b) -> p a b", b=128), func=AF.Copy),
             reads=b_ptbh, writes=[b_hT])

    def proj_fm(pool, w, bw, col0):
        pt, bp = pool.next()

        def mm(e):
            ins = None
            W = w.rearrange("p (k c) -> p k c", k=8)
            for kc in range(8):
                ins = e.matmul(pt[:], lhsT=W[:, kc, col0:col0 + 128], rhs=hT[:, kc, :], start=(kc == 0), stop=(kc == 7))
            return ins
        P.op(PE, mm, reads=[bw, b_hT], writes=[bp])
        return pt, bp

    def outproj(l, nkc, wname, kper, eps, nxt):
        P.tag = "L%d:out" % l
        ssA, ssB, rs = 16, 24, 32
        obank = [3, 4, 5, 6]
        wsrc = (lout_s if wname.startswith("lout") else rout_s)[int(wname[-1])]
        for half in range(2):
            for kg in range(2):
                w_, bw_ = wload("%s_%d" % (wname, half * 2 + kg), wsrc[half * 2 + kg], kper * 512)
                for j in range(NSUB):
                    po, bpo = pf[obank[j]], b_pf[obank[j]]

                    def mm(e, j=j, po=po, w_=w_, kg=kg):
                        ins = None
                        W = w_.rearrange("p (k c) -> p k c", c=512)
                        for kk in range(kper):
                            kc = kg * kper + kk
                            ins = e.matmul(po[:], lhsT=yT[:, kc, j * 128:(j + 1) * 128], rhs=W[:, kk, :],
                                           start=(kc == 0), stop=(kc == nkc - 1))
                        return ins
                    P.op(PE, mm, reads=[bw_] + b_yT[kg * kper:(kg + 1) * kper], writes=[bpo])
            for j in range(NSUB):
                po, bpo = pf[obank[j]], b_pf[obank[j]]
                if half == 0:
                    P.op(S_, lambda e, j=j, po=po: e.activation(out=otmp[:, j, :], in_=po[:], func=AF.Copy),
                         reads=[bpo], writes=[b_otmp[j]])
                    P.op(S_, lambda e, j=j, po=po: e.activation(out=junk[:, 0:512], in_=po[:], func=AF.Square, scale=1.0 / 32.0,
                                                                accum_out=stat[:, ssA + j:ssA + j + 1]),
                         reads=[bpo], writes=[b_junk, b_stat[ssA + j]])
                else:
                    P.op(S_, lambda e, j=j, po=po: e.activation(out=junk[:, 0:512], in_=po[:], func=AF.Square, scale=1.0 / 32.0,
                                                                accum_out=stat[:, ssB + j:ssB + j + 1]),
                         reads=[bpo], writes=[b_junk, b_stat[ssB + j]])
                    P.op(V, lambda e, j=j: e.scalar_tensor_tensor(out=stat[:, rs + j:rs + j + 1], in0=stat[:, ssA + j:ssA + j + 1], scalar=eps,
                                                                  in1=stat[:, ssB + j:ssB + j + 1], op0=ALU.add, op1=ALU.add),
                         reads=[b_stat[ssA + j], b_stat[ssB + j]], writes=[b_stat[rs + j]])
                    P.op(G, lambda e, j=j: e.tensor_tensor(out=stat[:, rs + j:rs + j + 1], in0=stat[:, rs + j:rs + j + 1], in1=cpow[:, 0:1], op=ALU.pow),
                         reads=[b_stat[rs + j], b_cpow], writes=[b_stat[rs + j]])
                    P.op(V, lambda e, j=j, po=po: e.scalar_tensor_tensor(out=po[:], in0=po[:], scalar=stat[:, rs + j:rs + j + 1],
                                                                         in1=gpost[:, 512:1024], op0=ALU.mult, op1=ALU.mult),
                         reads=[bpo, b_stat[rs + j], b_gpost], writes=[bpo])
                    P.op(V, lambda e, j=j, po=po: e.tensor_tensor(out=xt[:, j, 512:1024], in0=xt[:, j, 512:1024], in1=po[:], op=ALU.add),
                         reads=[bpo, b_xt[j]], writes=[b_xt[j]])
                    P.op(V, lambda e, j=j: e.scalar_tensor_tensor(out=otmp[:, j, :], in0=otmp[:, j, :], scalar=stat[:, rs + j:rs + j + 1],
                                                                  in1=gpost[:, 0:512], op0=ALU.mult, op1=ALU.mult),
                         reads=[b_otmp[j], b_stat[rs + j], b_gpost], writes=[b_otmp[j]])
                    P.op(V, lambda e, j=j: e.tensor_tensor(out=xt[:, j, 0:512], in0=xt[:, j, 0:512], in1=otmp[:, j, :], op=ALU.add),
                         reads=[b_otmp[j], b_xt[j]], writes=[b_xt[j]])
                    nxt.pre(j)
                    if j >= 1:
                        nxt.T(j - 1)
                    P.tag = "L%d:out" % l
        P.tag = "L%d:pre" % l
        nxt.T(NSUB - 1)

    def lru_layer(l, nxt):
        jj = l // 2
        lv, ld = lvec[jj], lder[jj]
        blv, bld = b_lvec[jj], b_lder[jj]
        dma(SY, "cst", wband[:], lband_s[jj].rearrange("p (g n) -> p g n", g=2), reads=[b_scr["lband%d" % jj]], writes=[b_wband])
        pool = Rot([0, 1, 2])
        gpool = Rot([3, 4])
        it = [0]

        def stageA(g):
            P.tag = "L%d:A" % l
            pb = g % 2
            xr, bxr, xcg, bxc, xb16, bx16 = xbraw[0], b_xbraw[0], xc[pb], b_xc[pb], xcbf[pb], b_xcbf[pb]
            w, bw = wload("lin%d_%d" % (jj, 2 * g), lin_s[jj, 2 * g], 3072)
            P.op(V, lambda e: e.tensor_copy(out=xr[:, :, 0:3], in_=halo[jj][:, 3 * g:3 * g + 3, :]),
                 reads=[b_halo[jj][g]], writes=[bxr])
            for ci in range(3):
                pt, bp = pool.next()

                def mm(e, pt=pt, ci=ci):
                    ins = None
                    W = w[:, 0:3072].rearrange("p (k c) -> p k c", k=8)
                    for kc in range(8):
                        ins = e.matmul(pt[:], lhsT=W[:, kc, ci * 128:(ci + 1) * 128], rhs=hT[:, kc, :], start=(kc == 0), stop=(kc == 7))
                    return ins
                P.op(PE, mm, reads=[bw, b_hT], writes=[bp])
                P.op(S_, lambda e, pt=pt, ci=ci: e.activation(out=xr[:, ci, 3:515], in_=pt[:], func=AF.Copy),
                     reads=[bp], writes=[bxr])
            P.op(V, lambda e: e.tensor_copy(out=halo[jj][:, 3 * g:3 * g + 3, :], in_=xr[:, :, 512:515]),
                 reads=[bxr], writes=[b_halo[jj][g]])
            for ci in range(3):
                c = 3 * g + ci
                P.op(S_, lambda e, ci=ci, c=c: e.activation(out=xcg[:, ci, :], in_=xr[:, ci, 0:512], func=AF.Identity,
                                                            scale=lv[:, c, 0:1], bias=lv[:, c, 4:5]),
                     reads=[bxr, blv], writes=[bxc])
                for k in range(1, 4):
                    P.op(V, lambda e, ci=ci, c=c, k=k: e.scalar_tensor_tensor(out=xcg[:, ci, :], in0=xr[:, ci, k:k + 512],
                                                                              scalar=lv[:, c, k:k + 1], in1=xcg[:, ci, :],
                                                                              op0=ALU.mult, op1=ALU.add),
                         reads=[bxr, blv, bxc], writes=[bxc])
            P.op(S_, lambda e: e.activation(out=xb16[:], in_=xcg[:], func=AF.Copy), reads=[bxc], writes=[bx16])

        def stageG(g):
            P.tag = "L%d:G" % l
            wg_, bwg = wload("lin%d_%d" % (jj, 2 * g + 1), lin_s[jj, 2 * g + 1], 3072)
            for co in range(3):
                tg_, btg = LT["tg"][co % 2]
                wgt, bwgt = LT["wg"][co]
                pgt, bpgt = pool.next()

                def mg(e, pgt=pgt, co=co):
                    ins = None
                    W = wg_[:, 0:3072].rearrange("p (k c) -> p k c", k=8)
                    for kc in range(8):
                        ins = e.matmul(pgt[:], lhsT=W[:, kc, co * 128:(co + 1) * 128], rhs=hT[:, kc, :], start=(kc == 0), stop=(kc == 7))
                    return ins
                P.op(PE, mg, reads=[bwg, b_hT], writes=[bpgt])
                P.op(S_, lambda e, pgt=pgt, tg_=tg_: e.activation(out=tg_[:], in_=pgt[:], func=AF.Tanh, scale=0.5), reads=[bpgt], writes=[btg])
                P.op(V, lambda e, pgt=pgt, tg_=tg_, wgt=wgt: e.scalar_tensor_tensor(out=wgt[:], in0=tg_[:], scalar=1.0, in1=pgt[:], op0=ALU.add, op1=ALU.mult),
                     reads=[btg, bpgt], writes=[bwgt])

        def stageB(g):
            P.tag = "L%d:B" % l
            pb = g % 2
            xb16, bx16 = xcbf[pb], b_xcbf[pb]
            for co in range(3):
                c = 3 * g + co
                sl = (3 * g + co) % NB
                tr_, btr = LT["tr"][co % 2]; ti_, bti = LT["ti"][sl]; a_, ba = LT["a"][sl]; m_, bm = LT["m"][sl]
                pa, bpa = gpool.next()
                px, bpx = gpool.next()
                for gi, (pg, bpg) in enumerate(((pa, bpa), (px, bpx))):
                    def gm(e, pg=pg, gi=gi, co=co):
                        ins = None
                        blks = [(bi, ci) for bi, (ci, co2) in enumerate(BL) if co2 == co]
                        for n, (bi, ci) in enumerate(blks):
                            o = (g * 7 + bi) * 128
                            ins = e.matmul(pg[:], lhsT=wband[:, gi, o:o + 128], rhs=xb16[:, ci, :], start=(n == 0), stop=(n == len(blks) - 1))
                        return ins
                    P.op(PE, gm, reads=[b_wband, bx16], writes=[bpg])
                P.op(S_, lambda e, pa=pa, tr_=tr_, c=c: e.activation(out=tr_[:], in_=pa[:], func=AF.Tanh, scale=0.5, bias=ld[:, c, 0:1]),
                     reads=[bpa, bld], writes=[btr])
                P.op(S_, lambda e, px=px, ti_=ti_, c=c: e.activation(out=ti_[:], in_=px[:], func=AF.Tanh, scale=0.5, bias=ld[:, c, 1:2]),
                     reads=[bpx, bld], writes=[bti])
                P.op(S_, lambda e, tr_=tr_, a_=a_, c=c: e.activation(out=a_[:], in_=tr_[:], func=AF.Exp, scale=ld[:, c, 2:3], bias=ld[:, c, 2:3]),
                     reads=[btr, bld], writes=[ba])
                P.op(S_, lambda e, tr_=tr_, m_=m_, c=c: e.activation(out=m_[:], in_=tr_[:], func=AF.Exp, scale=ld[:, c, 4:5], bias=ld[:, c, 5:6]),
                     reads=[btr, bld], writes=[bm])
                P.op(V, lambda e, m_=m_: e.tensor_scalar(out=m_[:], in0=m_[:], scalar1=QCLAMP, scalar2=None, op0=ALU.min),
                     reads=[bm], writes=[bm])

        def stageC(g):
            P.tag = "L%d:C" % l
            for co in range(3):
                m_, bm = LT["m"][(3 * g + co) % NB]
                P.op(S_, lambda e, m_=m_: e.activation(out=m_[:], in_=m_[:], func=AF.Sqrt, scale=-1.0, bias=0.25), reads=[bm], writes=[bm])

        def stageD(g):
            P.tag = "L%d:D" % l
            pb = g % 2
            xcg, bxc = xc[pb], b_xc[pb]
            for co in range(3):
                c = 3 * g + co
                tb = it[0] % 2
                it[0] += 1
                sl = (3 * g + co) % NB
                ti_, bti = LT["ti"][sl]; a_, ba = LT["a"][sl]; m_, bm = LT["m"][sl]
                u_, bu = LT["u"][tb]; hs_, bhs = LT["hs"][tb]; wgt, bwgt = LT["wg"][co]
                P.op(V, lambda e, ti_=ti_, u_=u_, co=co: e.scalar_tensor_tensor(out=u_[:], in0=ti_[:], scalar=1.0, in1=xcg[:, co, :],
                                                                               op0=ALU.add, op1=ALU.mult),
                     reads=[bti, bxc], writes=[bu])
                P.op(V, lambda e, u_=u_, m_=m_: e.tensor_tensor(out=u_[:], in0=u_[:], in1=m_[:], op=ALU.mult), reads=[bu, bm], writes=[bu])
                P.op(V, lambda e, a_=a_, u_=u_, hs_=hs_, c=c: e.tensor_tensor_scan(out=hs_[:], data0=a_[:], data1=u_[:], initial=hst[jj][:, c:c + 1],
                                                                                  op0=ALU.mult, op1=ALU.add),
                     reads=[ba, bu, b_hst[jj][c]], writes=[bhs])
                P.op(V, lambda e, hs_=hs_, c=c: e.tensor_copy(out=hst[jj][:, c:c + 1], in_=hs_[:, 511:512]), reads=[bhs], writes=[b_hst[jj][c]])
                P.op(V, lambda e, hs_=hs_, wgt=wgt, c=c: e.scalar_tensor_tensor(out=yT[:, c, :], in0=hs_[:], scalar=0.5, in1=wgt[:], op0=ALU.mult, op1=ALU.mult),
                     reads=[bhs, bwgt], writes=[b_yT[c]])

        stageA(0)
        for g in range(4):
            stageG(g)
            stageB(g)
            if g + 1 < 4:
                stageA(g + 1)
            stageC(g)
            stageD(g)
        outproj(l, 12, "lout%d" % jj, 6, RMS_EPS, nxt)

    def ret_layer(l, t, nxt):
        jj = l // 2
        S3 = S32[jj]
        bS3 = b_S32[jj]
        dma(SY, "cst", cosb[:], cos_d[:, t * T:(t + 1) * T], writes=[b_cos])
        dma(SY, "cst", sinb[:], sin_d[:, t * T:(t + 1) * T], writes=[b_sin])
        for k in range(8):
            P.op(S_, lambda e, k=k: e.activation(out=Sbf[:, k, :], in_=S3[:, k, :], func=AF.Copy), reads=[bS3[k]], writes=[b_Sbf[k]])
        pool = Rot([0, 1, 2, 3])
        def do_pair(pr):
            P.tag = "L%d:qkv" % l

            def rot_unit(dst, bdst, w, bw, hh):
                p1, bp1 = proj_fm(pool, w, bw, (2 * hh) * 128)
                p2, bp2 = proj_fm(pool, w, bw, (2 * hh + 1) * 128)
                t1, t2, t3, t4 = rt
                bt1, bt2, bt3, bt4 = b_rt
                P.op(V, lambda e: e.tensor_tensor(out=t1[:], in0=p1[:], in1=cosb[:], op=ALU.mult), reads=[bp1, b_cos], writes=[bt1])
                P.op(V, lambda e: e.tensor_tensor(out=t2[:], in0=p2[:], in1=sinb[:], op=ALU.mult), reads=[bp2, b_sin], writes=[bt2])
                P.op(V, lambda e: e.tensor_tensor(out=t3[:], in0=p2[:], in1=cosb[:], op=ALU.mult), reads=[bp2, b_cos], writes=[bt3])
                P.op(V, lambda e: e.tensor_tensor(out=t4[:], in0=p1[:], in1=sinb[:], op=ALU.mult), reads=[bp1, b_sin], writes=[bt4])
                P.op(V, lambda e: e.tensor_tensor(out=dst[:, 2 * hh, :], in0=t1[:], in1=t2[:], op=ALU.subtract),
                     reads=[bt1, bt2], writes=[bdst])
                P.op(V, lambda e: e.tensor_tensor(out=dst[:, 2 * hh + 1, :], in0=t3[:], in1=t4[:], op=ALU.add),
                     reads=[bt3, bt4], writes=[bdst])

            def v_piece(w, bw, hh, j):
                pv, bpv = pool.next()

                def mv(e):
                    ins = None
                    W = w.rearrange("p (k c) -> p k c", k=8)
                    for kc in range(8):
                        ins = e.matmul(pv[:], lhsT=hT[:, kc, j * 128:(j + 1) * 128], rhs=W[:, kc, :], start=(kc == 0), stop=(kc == 7))
                    return ins
                P.op(PE, mv, reads=[bw, b_hT], writes=[bpv])
                P.op(S_, lambda e: e.activation(out=vtok[:, j, hh * 512:(hh + 1) * 512], in_=pv[:], func=AF.Copy),
                     reads=[bpv], writes=[b_vtok])

            for qk, (dst, bdst) in enumerate(((qT, b_qT), (kT, b_kT))):
                w, bw = wload("rin%d_%d" % (jj, 2 * qk + pr), rin_s[jj, 2 * qk + pr], 4096)
                hv = 2 * pr + qk
                wv, bwv = wload("rin%d_%d" % (jj, 4 + hv), rin_s[jj, 4 + hv], 4096)
                for hh in range(2):
                    rot_unit(dst, bdst, w, bw, hh)
                    v_piece(wv, bwv, qk, 2 * hh)
                    v_piece(wv, bwv, qk, 2 * hh + 1)

            def gate_piece(s, mc):
                w, bw = gate_w[s]
                pg, bpg = proj_fm(gpool_r, w, bw, mc * 128)
                tb = mc % 2
                P.op(S_, lambda e: e.activation(out=rtg[tb][:], in_=pg[:], func=AF.Tanh, scale=0.5), reads=[bpg], writes=[b_rtg[tb]])
                P.op(V, lambda e: e.scalar_tensor_tensor(out=sgate[:, s * 4 + mc, :], in0=rtg[tb][:], scalar=1.0, in1=pg[:],
                                                         op0=ALU.add, op1=ALU.mult),
                     reads=[b_rtg[tb], bpg], writes=[b_sgate])
            gate_w = {}
            gpool_r = Rot([3, 7])
            P.tag = "L%d:kT" % l
            for j in range(NSUB):
                hb = j % 2

                def tk(e, j=j, hb=hb):
                    ins = None
                    for q in range(4):
                        ins = e.transpose(out=ptb[:, q * 128:(q + 1) * 128], in_=kT[:, q, j * 128:(j + 1) * 128], identity=ident[:])
                    return ins
                P.op(PE, tk, reads=[b_kT, b_ident], writes=b_ptbh)
                P.op(V, lambda e, j=j, pr=pr, hb=hb: e.tensor_tensor(out=ktok[:, j, :], in0=ptb[:, 0:512], in1=kdecb[:, pr * 512:(pr + 1) * 512], op=ALU.mult),
                     reads=b_ptbh + [b_rcon], writes=[b_ktok])
            P.tag = "L%d:chunk" % l
            zpool = Rot([0, 1, 2])
            spool = Rot([4, 6])
            sp, bsp = pf[5], b_pf[5]
            zb = {}

            def P1(j):
                pb = j % 2

                def ms(e):
                    ins = None
                    for hh in range(2):
                        for hf in range(2):
                            ins = e.matmul(sp[:, hh * 128:(hh + 1) * 128], lhsT=kT[:, 2 * hh + hf, j * 128:(j + 1) * 128],
                                           rhs=qT[:, 2 * hh + hf, j * 128:(j + 1) * 128], start=(hf == 0), stop=(hf == 1))
                    return ins
                P.op(PE, ms, reads=[b_kT, b_qT], writes=[bsp])
                P.op(V, lambda e: e.tensor_tensor(out=PT[pb][:].rearrange("p a b -> p (a b)"), in0=sp[:, 0:256],
                                                  in1=maskT[:, pr * 256:(pr + 1) * 256], op=ALU.mult),
                     reads=[bsp, b_rcon], writes=[b_PT[pb]])

            def P2(j):
                pb = j % 2
                for hh in range(2):
                    h = 2 * pr + hh
                    pz, bpz = zpool.next()
                    zb[(j, hh)] = (pz, bpz)

                    def mz(e, pz=pz, hh=hh, h=h):
                        e.matmul(pz[:], lhsT=PT[pb][:, hh, :], rhs=vtok[:, j, hh * 512:(hh + 1) * 512], start=True, stop=False)
                        e.matmul(pz[:], lhsT=qT[:, 2 * hh, j * 128:(j + 1) * 128], rhs=Sbf[:, 2 * h, :], start=False, stop=False)
                        return e.matmul(pz[:], lhsT=qT[:, 2 * hh + 1, j * 128:(j + 1) * 128], rhs=Sbf[:, 2 * h + 1, :], start=False, stop=True)
                    P.op(PE, mz, reads=[b_PT[pb], b_vtok, b_qT, b_Sbf[2 * h], b_Sbf[2 * h + 1]], writes=[bpz])
                for hh in range(2):
                    h = 2 * pr + hh
                    for hf in range(2):
                        k = 2 * h + hf
                        pst, bpst = spool.next()
                        P.op(PE, lambda e, pst=pst, hh=hh, hf=hf: e.matmul(pst[:], lhsT=ktok[:, j, hh * 256 + hf * 128: hh * 256 + (hf + 1) * 128],
                                                                          rhs=vtok[:, j, hh * 512:(hh + 1) * 512], start=True, stop=True),
                             reads=[b_ktok, b_vtok], writes=[bpst])
                        P.op(V, lambda e, pst=pst, k=k, h=h: e.scalar_tensor_tensor(out=S3[:, k, :], in0=S3[:, k, :], scalar=float(GAM[h] ** 128), in1=pst[:],
                                                                                  op0=ALU.mult, op1=ALU.add),
                             reads=[bpst, bS3[k]], writes=[bS3[k]])
                        P.op(S_, lambda e, k=k: e.activation(out=Sbf[:, k, :], in_=S3[:, k, :], func=AF.Copy), reads=[bS3[k]], writes=[b_Sbf[k]])
                for hh in range(2):
                    pz, bpz = zb[(j, hh)]
                    o6 = 40 + hh * 6
                    omv = 52 + hh * 2
                    P.op(V, lambda e, pz=pz, o6=o6: e.bn_stats(out=stat[:, o6:o6 + 6], in_=pz[:]), reads=[bpz], writes=[b_stat[o6]])
                    P.op(V, lambda e, o6=o6, omv=omv: e.bn_aggr(out=stat[:, omv:omv + 2], in_=stat[:, o6:o6 + 6]), reads=[b_stat[o6]], writes=[b_stat[omv]])

            def P3(j):
                pb = j
                mvv = stat[:, 52:56].rearrange("p (h t) -> p h t", t=2)
                P.op(G, lambda e: e.tensor_tensor(out=stat[:, 56:58], in0=mvv[:, :, 1], in1=epsc[:, 2 * pr:2 * pr + 2], op=ALU.add),
                     reads=[b_stat[52], b_stat[54], b_rcon], writes=[b_stat[56]])
                P.op(G, lambda e: e.tensor_tensor(out=stat[:, 56:58], in0=stat[:, 56:58], in1=cpow[:, 0:2], op=ALU.pow),
                     reads=[b_stat[56], b_cpow], writes=[b_stat[56]])
                P.op(V, lambda e: e.scalar_tensor_tensor(out=stat[:, 58:60], in0=mvv[:, :, 0], scalar=-1.0, in1=stat[:, 56:58], op0=ALU.mult, op1=ALU.mult),
                     reads=[b_stat[52], b_stat[54], b_stat[56]], writes=[b_stat[58]])
                for hh in range(2):
                    pz, bpz = zb[(j, hh)]
                    P.op(S_, lambda e, pz=pz, hh=hh: e.activation(out=gntok[pb][:, hh * 512:(hh + 1) * 512], in_=pz[:], func=AF.Identity,
                                                                  scale=stat[:, 56 + hh:57 + hh], bias=stat[:, 58 + hh:59 + hh]),
                         reads=[bpz, b_stat[56], b_stat[58]], writes=[b_gntok[pb]])

            def P4(j):
                pb = j

                tbk, btbk = (ptb, b_ptbh) if j % 2 == 0 else (pf3_bf, [b_pf[3]])

                def tg(e):
                    ins = None
                    for q in range(8):
                        ins = e.transpose(out=tbk[:, q * 128:(q + 1) * 128], in_=gntok[pb][:, q * 128:(q + 1) * 128], identity=ident[:])
                    return ins
                P.op(PE, tg, reads=[b_gntok[pb], b_ident], writes=btbk)
                P.op(V, lambda e: e.tensor_tensor(out=yT[:, pr * 8:(pr + 1) * 8, j * 128:(j + 1) * 128],
                                                  in0=tbk[:].rearrange("p (a b) -> p a b", b=128),
                                                  in1=sgate[:, :, j * 128:(j + 1) * 128], op=ALU.mult),
                     reads=btbk + [b_sgate], writes=b_yT[pr * 8:(pr + 1) * 8])

            P1(0)
            for j in range(NSUB):
                P2(j)
                if j + 1 < NSUB:
                    P1(j + 1)
                P.tag = "L%d:gate" % l
                s_ = j // 2
                if j % 2 == 0:
                    gate_w[s_] = wload("rin%d_%d" % (jj, 8 + 2 * pr + s_), rin_s[jj, 8 + 2 * pr + s_], 4096)
                gate_piece(s_, 2 * (j % 2))
                gate_piece(s_, 2 * (j % 2) + 1)
                P.tag = "L%d:chunk" % l
                P3(j)
            for j in range(NSUB):
                P4(j)
        for pr_ in range(2):
            do_pair(pr_)
        outproj(l, 16, "rout%d" % jj, 8, RMS_EPS / 4.0, nxt)

    steps = [(sq, t, l) for sq in range(NSEQ) for t in range(NT) for l in layers]

    def xrows(sq, t, j):
        r0 = sq * SEQ + t * T + j * 128
        return r0, r0 + 128

    class Nxt:
        def __init__(self, k):
            self.k = k
            self.cur = steps[k]
            self.nx = steps[k + 1] if k + 1 < len(steps) else None

        def pre(self, j):
            P.tag = "L%d:pre" % self.cur[2]
            sq, t, l = self.cur
            if l == layers[-1]:
                a, b = xrows(sq, t, j)
                dma(SY, "st", out_d[a:b, :], xt[:, j, :], reads=[b_xt[j]])
                if self.nx is not None:
                    a, b = xrows(self.nx[0], self.nx[1], j)
                    dma(SY, "xl", xt[:, j, :], x_d[a:b, :], writes=[b_xt[j]])
            if self.nx is not None:
                if j == 0:
                    dma(SY, "cst", gpre[:], gpre_d[self.nx[2]], writes=[b_gpre])
                pre_chain(j)

        def T(self, j):
            if self.nx is not None:
                pre_T(j)

    for j in range(NSUB):
        a, b = xrows(0, 0, j)
        dma(SY, "xl", xt[:, j, :], x_d[a:b, :], writes=[b_xt[j]])
    dma(SY, "cst", gpre[:], gpre_d[layers[0]], writes=[b_gpre])
    for j in range(NSUB):
        pre_chain(j)
        pre_T(j)
    for k, (sq, t, l) in enumerate(steps):
        jj = l // 2
        if t == 0:
            if l % 2 == 0:
                P.op(V, lambda e, jj=jj: e.memset(hst[jj][:], 0.0), writes=b_hst[jj])
                P.op(V, lambda e, jj=jj: e.memset(halo[jj][:], 0.0), writes=b_halo[jj])
            else:
                P.op(G, lambda e, jj=jj: e.memset(S32[jj][:], 0.0), writes=b_S32[jj])
        dma(SY, "cst", gpost[:], gpost_d[l], writes=[b_gpost])
        P.tag = "T%d.L%d" % (t, l)
        if l % 2 == 0:
            lru_layer(l, Nxt(k))
        else:
            ret_layer(l, t, Nxt(k))
    P.wait_all(SY, ["st"])

    if os.environ.get("K_MEM"):
        print("SBUF remaining", nc.sbuf_bytes_remaining)
    P.finalize(reorder=not os.environ.get("K_NO_REORDER"))
    sems = {}
    for k in P.cnt:
        sems[k] = es.enter_context(nc.semaphore("s_" + k.replace(".", "_")))
    with nc.Block() as block:
        def emit(eng_name):
            def body(e):
                for deps, fn, key, inc in P.q[eng_name]:
                    for k, v in deps:
                        e.wait_ge(sems[k], v)
                    if fn is not None:
                        ins = fn(e)
                        ins.then_inc(sems[key], inc)
            return body
        if os.environ.get("KDUMP_TAGS"):
            class Cnt:
                def __init__(self):
                    self.n = 0
                def matmul(self, *a, **k):
                    self.n += 1
                    return self
                transpose = matmul
            rows = []
            for (deps, fn, key, inc), tg in zip(P.q["tensor"], P.tags["tensor"]):
                if fn is None:
                    continue
                c = Cnt()
                fn(c)
                rows.append((tg, c.n))
            import json
            json.dump(rows, open(os.environ["KDUMP_TAGS"], "w"))
        block.tensor(emit("tensor"))
        block.vector(emit("vector"))
        block.scalar(emit("scalar"))
        block.gpsimd(emit("gpsimd"))
        block.sync(emit("sync"))
    es.close()
    return nc


def _consts(SEQ):
    pos = np.arange(SEQ, dtype=np.float32)
    inv_freq = (10000.0 ** (-np.arange(0, 256, 2, dtype=np.float32) / 256)).astype(np.float32)
    ang = (pos[:, None] * inv_freq[None, :]).astype(np.float32)
    cosT = np.ascontiguousarray(np.cos(ang).T.astype(np.float32))
    sinT = np.ascontiguousarray(np.sin(ang).T.astype(np.float32))
    idx = np.arange(128, dtype=np.float64)
    rcon = np.zeros((128, 512 + 1024 + 4), np.float32)
    for h in range(4):
        g = GAM[h]
        m = (g ** (-(idx[:, None] + 1.0))) * (idx[None, :] >= idx[:, None]) * (256 ** -0.5)
        rcon[:, h * 128:(h + 1) * 128] = m
        rcon[:, 512 + h * 256: 512 + (h + 1) * 256] = ((g ** (127.0 - idx)) * (256 ** -0.5))[:, None]
        rcon[:, 1536 + h] = GN_EPS * g ** (-2.0 * (idx + 1.0))
    ident = np.eye(128, dtype=np.float32).astype(ml_dtypes.bfloat16)
    return cosT, sinT, rcon, ident


def _layout_weights(inp):
    f = np.float32
    lw_in = np.asarray(inp["lru_w_in"], f)
    lin = np.zeros((2, 8, 128, 3072), f)
    for j in range(2):
        w = lw_in[j].reshape(8, 128, 3072)
        for g in range(4):
            for br in range(2):
                cols = w[:, :, br * 1536 + g * 384: br * 1536 + (g + 1) * 384]
                lin[j, 2 * g + br] = cols.transpose(1, 0, 2).reshape(128, 3072)
    lw_out = np.asarray(inp["lru_w_out"], f)
    lout = np.zeros((2, 4, 128, 3072), f)
    for j in range(2):
        w = lw_out[j].reshape(2, 6, 128, 2, 512)
        for half in range(2):
            for kg in range(2):
                lout[j, half * 2 + kg] = w[kg, :, :, half, :].transpose(1, 0, 2).reshape(128, 3072)
    lband = np.zeros((2, 128, 2, 4, 7, 128), f)
    for j in range(2):
        for gi, nm in enumerate(("lru_w_a", "lru_w_x")):
            wa = np.asarray(inp[nm], f)[j]
            for g in range(4):
                full = np.zeros((384, 384), f)
                for n in range(4):
                    full[96 * n:96 * n + 96, 96 * n:96 * n + 96] = wa[4 * g + n]
                for bi, (ci, co) in enumerate(BL):
                    lband[j, :, gi, g, bi, :] = full[ci * 128:(ci + 1) * 128, co * 128:(co + 1) * 128]
    lband = lband.reshape(2, 128, 2 * 3584)
    lvec = np.zeros((2, 128, 12, 9), f)
    for j in range(2):
        cw = np.asarray(inp["lru_conv_w"], f)[j]
        for k in range(4):
            lvec[j, :, :, k] = cw[k].reshape(12, 128).T
        for k, nm in enumerate(("lru_conv_b", "lru_b_a", "lru_b_x", "lru_lambda")):
            lvec[j, :, :, 4 + k] = np.asarray(inp[nm], f)[j].reshape(12, 128).T
    lvec = lvec.reshape(2, 128, 108)
    rw_in = np.asarray(inp["ret_w_in"], f)
    rin = np.zeros((2, 12, 128, 4096), f)
    for j in range(2):
        w = rw_in[j].reshape(8, 128, 12, 512)
        rin[j] = w.transpose(2, 1, 0, 3).reshape(12, 128, 4096)
    rw_out = np.asarray(inp["ret_w_out"], f)
    rout = np.zeros((2, 4, 128, 4096), f)
    for j in range(2):
        w = rw_out[j].reshape(2, 8, 128, 2, 512)
        for half in range(2):
            for kg in range(2):
                rout[j, half * 2 + kg] = w[kg, :, :, half, :].transpose(1, 0, 2).reshape(128, 4096)
    gpre = np.ascontiguousarray(np.broadcast_to(np.asarray(inp["norm_pre"], f)[:, None, :], (4, 128, D)))
    gpost = np.ascontiguousarray(np.broadcast_to(np.asarray(inp["norm_post"], f)[:, None, :], (4, 128, D)))
    return dict(gpre=gpre, gpost=gpost, lru_in=lin, lru_out=lout, lru_band=lband, lru_vec=lvec, ret_in=rin, ret_out=rout)


def run(inputs, ncores=NCORE, layers=(0, 1, 2, 3)):
    x = np.asarray(inputs["x"], np.float32)
    B, SEQ, _ = x.shape
    nseq = B // ncores
    nc = build(nseq, SEQ, list(layers))
    shared = _layout_weights(inputs)
    cosT, sinT, rcon, ident = _consts(SEQ)
    shared.update(cosT=cosT, sinT=sinT, rcon=rcon, ident=ident)
    in_maps = []
    for c in range(ncores):
        m = dict(shared)
        m["x"] = np.ascontiguousarray(x[c * nseq:(c + 1) * nseq].reshape(nseq * SEQ, D))
        in_maps.append(m)
    res = run_bass_kernel_spmd(nc, in_maps, core_ids=list(range(ncores)))
    out = np.stack([r["out"].reshape(nseq, SEQ, D) for r in res.results], axis=0).reshape(B, SEQ, D)
    return out.astype(np.float32)


def kernel(**inputs):
    return run(inputs)
```
